# Optimizing a Trainium2 kernel written in Bass

```python
import jax, jax.numpy as jnp
from jax import lax
import numpy as np

D_MODEL = 2048
BATCH = 4
SEQ = 8192
DEPTH = 1

HEAD_DIM = 128
N_HEADS_MOBA = 8
N_HEADS_DIL = 8
W_MOBA = N_HEADS_MOBA * HEAD_DIM
W_DIL = N_HEADS_DIL * HEAD_DIM
MOBA_BLOCK = 256
MOBA_TOPK = 3
MOBA_Q_CHUNK = 32
DIL_PATTERNS = ((128, 1), (512, 4), (2048, 16))
N_GROUPS = 4
EXPERTS_PER_GROUP = 8
N_EXPERTS = N_GROUPS * EXPERTS_PER_GROUP
EXPERT_TOPK = 2
D_EXPERT = D_MODEL // 4
MOE_BLOCK = 128
RMS_EPS = 1e-6
IN_COLS = 3 * W_MOBA + 3 * W_DIL + 2 * D_MODEL

kernel_name = "hybrid_moba_dilated_hmoe_block"


def rms_norm(x, g):
    xf = x.astype(jnp.float32)
    y = xf * lax.rsqrt(jnp.mean(xf * xf, axis=-1, keepdims=True) + RMS_EPS)
    return (y * g.astype(jnp.float32)).astype(x.dtype)


def alibi_slopes():
    n = N_HEADS_MOBA + N_HEADS_DIL
    s = 2.0 ** (-8.0 * np.arange(1, n + 1) / n)
    return jnp.asarray(s[0::2], jnp.float32), jnp.asarray(s[1::2], jnp.float32)


def masked_softmax_lse(s, mask):
    s = jnp.where(mask, s, -jnp.inf)
    m = jnp.max(s, axis=-1, keepdims=True)
    m = jnp.where(jnp.isfinite(m), m, 0.0)
    p = jnp.exp(s - m)
    l = jnp.sum(p, axis=-1, keepdims=True)
    lse = (jnp.log(l) + m)[..., 0]
    p = p / jnp.where(l > 0, l, 1.0)
    return p, lse


def merge_by_lse(outs, lses):
    w = jax.nn.softmax(lses, axis=0)
    return jnp.sum(w[..., None] * outs.astype(jnp.float32), axis=0).astype(outs.dtype)


def moba_attention(q, k, v, slopes):
    Bsz, H, S, HD = q.shape
    nb = -(-S // MOBA_BLOCK)
    sp = nb * MOBA_BLOCK
    pad = ((0, 0), (0, 0), (0, sp - S), (0, 0))
    q, k, v = (jnp.pad(a, pad) for a in (q, k, v))
    scale = HD ** -0.5
    qb = q.reshape(Bsz, H, nb, MOBA_BLOCK, HD)
    kb = k.reshape(Bsz, H, nb, MOBA_BLOCK, HD)
    vb = v.reshape(Bsz, H, nb, MOBA_BLOCK, HD)

    i = jnp.arange(MOBA_BLOCK)
    diff = (i[:, None] - i[None, :])
    s_own = (jnp.einsum('bhnqd,bhnkd->bhnqk', qb, kb).astype(jnp.float32) * scale
             - slopes[None, :, None, None, None] * diff.astype(jnp.float32))
    p_own, lse_own = masked_softmax_lse(s_own, diff >= 0)
    o_own = jnp.einsum('bhnqk,bhnkd->bhnqd', p_own.astype(v.dtype), vb).reshape(Bsz, H, sp, HD)
    lse_own = lse_own.reshape(Bsz, H, sp)

    k_mean = jnp.mean(kb.astype(jnp.float32), axis=3)
    pos = jnp.arange(sp)
    qblk = pos // MOBA_BLOCK
    gate = jnp.einsum('bhsd,bhnd->bhsn', q.astype(jnp.float32), k_mean)
    past = jnp.arange(nb)[None, :] < qblk[:, None]
    gate = jnp.where(past, gate, -jnp.inf)
    n_sel = min(MOBA_TOPK, nb)
    _, sel = lax.top_k(gate, n_sel)
    sel_valid = sel < qblk[:, None]

    nc = sp // MOBA_Q_CHUNK

    def to_chunks(a):
        a = a.reshape((Bsz, H, nc, MOBA_Q_CHUNK) + a.shape[3:])
        return jnp.moveaxis(a, 2, 0)

    b_idx = jnp.arange(Bsz)[:, None, None, None]
    h_idx = jnp.arange(H)[None, :, None, None]
    offs = jnp.arange(MOBA_BLOCK)

    def chunk_attend(args):
        q_c, sel_c, valid_c, t_c = args
        k_g = kb[b_idx, h_idx, sel_c]
        v_g = vb[b_idx, h_idx, sel_c]
        s = jnp.einsum('bhcd,bhcnjd->bhcnj', q_c, k_g).astype(jnp.float32) * scale
        key_pos = sel_c[..., None] * MOBA_BLOCK + offs
        dist = (t_c[None, None, :, None, None] - key_pos).astype(jnp.float32)
        s = s - slopes[None, :, None, None, None] * dist
        mask = jnp.broadcast_to(valid_c[..., None], s.shape)
        C = q_c.shape[2]
        s = s.reshape(Bsz, H, C, n_sel * MOBA_BLOCK)
        mask = mask.reshape(Bsz, H, C, n_sel * MOBA_BLOCK)
        p, lse = masked_softmax_lse(s, mask)
        o = jnp.einsum('bhcm,bhcmd->bhcd', p.astype(v_g.dtype),
                       v_g.reshape(Bsz, H, C, n_sel * MOBA_BLOCK, HD))
        return o, lse

    o_sel, lse_sel = lax.map(chunk_attend, (to_chunks(q), to_chunks(sel), to_chunks(sel_valid),
                                            pos.reshape(nc, MOBA_Q_CHUNK)))
    o_sel = jnp.moveaxis(o_sel, 0, 2).reshape(Bsz, H, sp, HD)
    lse_sel = jnp.moveaxis(lse_sel, 0, 2).reshape(Bsz, H, sp)
    o = merge_by_lse(jnp.stack([o_own, o_sel]), jnp.stack([lse_own, lse_sel]))
    return o[:, :, :S]


def dilated_attention_one(q, k, v, slopes, window, dilation):
    Bsz, H, S, HD = q.shape
    w = window // dilation
    span = dilation * w
    sp = -(-S // span) * span
    L = sp // dilation
    nb = L // w
    scale = HD ** -0.5
    pad = ((0, 0), (0, 0), (0, sp - S), (0, 0))

    def to_blocks(a):
        a = jnp.pad(a, pad).reshape(Bsz, H, L, dilation, HD)
        a = jnp.swapaxes(a, 2, 3)
        return a.reshape(Bsz, H, dilation, nb, w, HD)

    def with_prev(a):
        prev = jnp.pad(a, ((0, 0), (0, 0), (0, 0), (1, 0), (0, 0), (0, 0)))[:, :, :, :-1]
        return jnp.concatenate([prev, a], axis=4)

    qb = to_blocks(q)
    kk = with_prev(to_blocks(k))
    vv = with_prev(to_blocks(v))
    i = jnp.arange(w)[:, None]
    j = jnp.arange(2 * w)[None, :]
    diff = i + w - j
    blk = jnp.arange(nb)[:, None, None]
    mask = ((diff >= 0) & (diff <= w))[None] & ((blk > 0) | (j >= w)[None])
    s = (jnp.einsum('bhrnqd,bhrnkd->bhrnqk', qb, kk).astype(jnp.float32) * scale
         - slopes[None, :, None, None, None, None] * (dilation * diff).astype(jnp.float32))
    p, lse = masked_softmax_lse(s, mask)
    o = jnp.einsum('bhrnqk,bhrnkd->bhrnqd', p.astype(vv.dtype), vv)
    o = jnp.swapaxes(o.reshape(Bsz, H, dilation, L, HD), 2, 3).reshape(Bsz, H, sp, HD)
    lse = jnp.swapaxes(lse.reshape(Bsz, H, dilation, L), 2, 3).reshape(Bsz, H, sp)
    return o[:, :, :S], lse[:, :, :S]


def dilated_mixture(q, k, v, slopes):
    outs, lses = [], []
    for window, dilation in DIL_PATTERNS:
        o, lse = dilated_attention_one(q, k, v, slopes, window, dilation)
        outs.append(o)
        lses.append(lse)
    return merge_by_lse(jnp.stack(outs), jnp.stack(lses))


def hier_moe(h, w_group, b_group, w_expert, b_expert, w_gate, w_up, w_down):
    Bsz, S, D = h.shape
    N = Bsz * S
    hf = h.reshape(N, D)
    g_prob = jax.nn.softmax((hf @ w_group + b_group).astype(jnp.float32), axis=-1)
    g_top_p, g_top = lax.top_k(g_prob, 1)
    e_logits = (hf @ w_expert + b_expert).astype(jnp.float32).reshape(N, N_GROUPS, EXPERTS_PER_GROUP)
    e_logits = jnp.take_along_axis(e_logits, g_top[:, :, None], axis=1)[:, 0]
    e_prob = jax.nn.softmax(e_logits, axis=-1)
    e_top_p, e_top = lax.top_k(e_prob, EXPERT_TOPK)
    weights = g_top_p * e_top_p / jnp.sum(e_top_p, axis=-1, keepdims=True)
    expert_id = g_top * EXPERTS_PER_GROUP + e_top

    A = N * EXPERT_TOPK
    e_flat = expert_id.reshape(A).astype(jnp.int32)
    w_flat = weights.reshape(A).astype(h.dtype)
    tok_flat = jnp.repeat(jnp.arange(N, dtype=jnp.int32), EXPERT_TOPK)
    order = jnp.argsort(e_flat)
    e_sorted = e_flat[order]
    counts = jnp.bincount(e_flat, length=N_EXPERTS)
    padded = ((counts + MOE_BLOCK - 1) // MOE_BLOCK) * MOE_BLOCK
    start = jnp.cumsum(counts) - counts
    pend = jnp.cumsum(padded)
    pstart = pend - padded
    dest = pstart[e_sorted] + (jnp.arange(A) - start[e_sorted])
    R = -(-(A + N_EXPERTS * MOE_BLOCK) // MOE_BLOCK) * MOE_BLOCK
    n_blk = R // MOE_BLOCK
    buf_tok = jnp.zeros((R,), jnp.int32).at[dest].set(tok_flat[order])
    buf_w = jnp.zeros((R,), h.dtype).at[dest].set(w_flat[order])
    blk_start = jnp.arange(n_blk) * MOE_BLOCK
    blk_e = jnp.clip(jnp.sum(pend[None, :] <= blk_start[:, None], axis=1), 0, N_EXPERTS - 1)
    xb = hf[buf_tok].reshape(n_blk, MOE_BLOCK, D)

    def expert_block(args):
        xblk, e = args
        return (jax.nn.silu(xblk @ w_gate[e]) * (xblk @ w_up[e])) @ w_down[e]

    yb = lax.map(expert_block, (xb, blk_e)).reshape(R, D)
    out = jnp.zeros((N, D), h.dtype).at[buf_tok].add(yb * buf_w[:, None])
    return out.reshape(Bsz, S, D)


def setup_inputs(seed: int = 0) -> dict:
    key = jax.random.key(seed)
    ks = jax.random.split(key, 16)
    D = D_MODEL
    L = DEPTH

    def nrm(k, shape, scale):
        return jax.random.normal(k, shape, jnp.float32) * scale

    return {
        "x": nrm(ks[0], (BATCH, SEQ, D), 1.0),
        "norm1_g": 1.0 + nrm(ks[1], (L, D), 0.02),
        "w_in": nrm(ks[2], (L, D, IN_COLS), D ** -0.5),
        "b_gates": nrm(ks[3], (L, 2 * D), 0.01),
        "w_out_moba": nrm(ks[4], (L, W_MOBA, D), W_MOBA ** -0.5),
        "w_out_dil": nrm(ks[5], (L, W_DIL, D), W_DIL ** -0.5),
        "w_o": nrm(ks[6], (L, D, D), D ** -0.5),
        "norm2_g": 1.0 + nrm(ks[7], (L, D), 0.02),
        "w_group": nrm(ks[8], (L, D, N_GROUPS), D ** -0.5),
        "b_group": nrm(ks[9], (L, N_GROUPS), 0.01),
        "w_expert": nrm(ks[10], (L, D, N_EXPERTS), D ** -0.5),
        "b_expert": nrm(ks[11], (L, N_EXPERTS), 0.01),
        "w_gate": nrm(ks[12], (L, N_EXPERTS, D, D_EXPERT), D ** -0.5),
        "w_up": nrm(ks[13], (L, N_EXPERTS, D, D_EXPERT), D ** -0.5),
        "w_down": nrm(ks[14], (L, N_EXPERTS, D_EXPERT, D), D_EXPERT ** -0.5),
        "norm_f_g": 1.0 + nrm(ks[15], (D,), 0.02),
    }


def reference(x, norm1_g, w_in, b_gates, w_out_moba, w_out_dil, w_o, norm2_g, w_group, b_group,
              w_expert, b_expert, w_gate, w_up, w_down, norm_f_g):
    slopes_moba, slopes_dil = alibi_slopes()
    Bsz, S, D = x.shape
    cuts = list(np.cumsum([W_MOBA, W_MOBA, W_MOBA, W_DIL, W_DIL, W_DIL, D_MODEL]))

    def heads(a, n):
        return a.reshape(Bsz, S, n, HEAD_DIM).transpose(0, 2, 1, 3)

    def merge_heads(a):
        return a.transpose(0, 2, 1, 3).reshape(Bsz, S, -1)

    for l in range(DEPTH):
        h = rms_norm(x, norm1_g[l])
        proj = h @ w_in[l]
        q_m, k_m, v_m, q_d, k_d, v_d, g_m, g_d = jnp.split(proj, cuts, axis=-1)
        o_m = moba_attention(heads(q_m, N_HEADS_MOBA), heads(k_m, N_HEADS_MOBA),
                             heads(v_m, N_HEADS_MOBA), slopes_moba)
        o_d = dilated_mixture(heads(q_d, N_HEADS_DIL), heads(k_d, N_HEADS_DIL),
                              heads(v_d, N_HEADS_DIL), slopes_dil)
        y_m = merge_heads(o_m) @ w_out_moba[l]
        y_d = merge_heads(o_d) @ w_out_dil[l]
        gates = jax.nn.sigmoid(jnp.concatenate([g_m, g_d], axis=-1) + b_gates[l])
        gate_m, gate_d = jnp.split(gates, 2, axis=-1)
        x = x + (gate_m * y_m + gate_d * y_d) @ w_o[l]
        x = x + hier_moe(rms_norm(x, norm2_g[l]), w_group[l], b_group[l], w_expert[l], b_expert[l],
                         w_gate[l], w_up[l], w_down[l])
    return rms_norm(x, norm_f_g)
```

```python
import numpy as np
import ml_dtypes
from contextlib import ExitStack
import concourse.bass as bass
import concourse.mybir as mybir
from concourse.bass_utils import run_bass_kernel_spmd

F32 = mybir.dt.float32
BF16 = mybir.dt.bfloat16
I32 = mybir.dt.int32
AF = mybir.ActivationFunctionType
ALU = mybir.AluOpType
AX = mybir.AxisListType

D = 2048
S = 8192
HD = 128
NH = 8
NT = 64
OWN = 4096
NEXP = 32
DE = 512
CAP = 384
NSLOT = NEXP * CAP
EPS = 1e-6
NEG = -30000.0
SCALE = HD ** -0.5
IN_COLS = 10240


def _slopes():
    n = 16
    s = 2.0 ** (-8.0 * np.arange(1, n + 1) / n)
    return s[0::2].astype(np.float64), s[1::2].astype(np.float64)


class Buf:
    def __init__(self, t, ch=None):
        self.t = t
        self.wr = {}
        self.rd = {}
        self.ch = ch

    def w(self, tok, first=True):
        if first:
            self.wr = {}
            self.rd = {}
        self.wr[tok[2]] = tok

    def r(self, tok):
        self.rd[tok[2]] = tok

    def RD(self):
        return list(self.wr.values())

    def WR(self):
        return list(self.rd.values()) + list(self.wr.values())


class Chan:
    def __init__(self, sem, key):
        self.sem = sem
        self.key = key
        self.cnt = 0


class Ker:
    def __init__(self, nc, es):
        self.nc = nc
        self.es = es
        self.E = dict(pe=nc.tensor, act=nc.scalar, dve=nc.vector, pool=nc.gpsimd, sp=nc.sync)
        self.sem = {}
        self.cnt = {}
        for k in ["pe", "act", "dve", "pool"]:
            self.sem[k] = es.enter_context(nc.semaphore("S" + k))
            self.cnt[k] = 0
        self.seen = {}
        self.nch = 0
        self.stores = {}

    def chan(self):
        self.nch += 1
        key = "c%d" % self.nch
        return Chan(self.es.enter_context(self.nc.semaphore("C%d" % self.nch)), key)

    def _flat(self, deps, out):
        for d in deps:
            if d is None:
                continue
            if isinstance(d, (list,)) or (isinstance(d, tuple) and (len(d) != 3 or not isinstance(d[1], int))):
                self._flat(d, out)
            else:
                out.append(d)

    def wait(self, e, deps):
        fl = []
        self._flat(deps, fl)
        for sem, val, key in fl:
            if self.seen.get((e, key), 0) >= val:
                continue
            self.E[e].wait_ge(sem, val)
            self.seen[(e, key)] = val

    def op(self, e, fn, deps=()):
        self.wait(e, deps)
        ins = fn(self.E[e])
        self.cnt[e] += 1
        ins.then_inc(self.sem[e], 1)
        return (self.sem[e], self.cnt[e], e)

    def dma(self, ch, q, out, in_, deps=(), store=False):
        self.wait(q, deps)
        ins = self.E[q].dma_start(out=out, in_=in_)
        ch.cnt += 16
        ins.then_inc(ch.sem, 16)
        tok = (ch.sem, ch.cnt, ch.key)
        if store:
            self.stores[ch.key] = tok
        return tok

    def barrier(self, q):
        self.wait(q, list(self.stores.values()))


def build_program(dbg=None):
    nc = bass.Bass("TRN2", target_bir_lowering=False)
    dbg = dbg or set()

    def din(name, shape, dt=F32):
        if "small" in dbg and name in ("xl", "w_in", "w_gate", "w_up", "w_down", "w_om", "w_od", "w_o"):
            return None
        return nc.dram_tensor(name, list(shape), dt, kind="ExternalInput").ap()

    def dscr(name, shape, dt):
        kind = "ExternalOutput" if name in dbg else "Internal"
        if ("in:" + name) in dbg:
            kind = "ExternalInput"
        return nc.dram_tensor(name, list(shape), dt, kind=kind).ap()

    xl = din("xl", [S, D])
    w_in = din("w_in", [D, IN_COLS])
    g1rep = din("g1rep", [128, D])
    bg = din("bg", [128, 32])
    ident_d = din("ident", [128, 128], BF16)
    identf_d = din("identf", [128, 128], F32)
    ab_d = din("ab", [128, NH * 64])
    ef_d = din("ef", [128, NH * 2])
    bown_d = din("bown", [128, NH * 256])
    pbq_d = din("pbq", [128, 32 * 32])
    pvq_d = din("pvq", [128, 32 * 32])
    esel_d = din("esel", [128, 2 * 32 * 128], BF16)
    slp_d = din("slp", [NH, 2, OWN], BF16)
    valid_d = din("valid", [128, NT], BF16)
    rg_d = din("rg", [NH, 128, 8 * 2 * 256], BF16)
    bownd_d = din("bownd", [128, NH * 384])
    abd_d = din("abd", [128, NH * 17])
    efd_d = din("efd", [128, NH * 2])
    ctl_d = din("ctl", [128, 3 * 512], BF16)
    lsd_d = din("lsd", [128, 2 * 128], BF16)
    w_om = din("w_om", [1024, D])
    w_od = din("w_od", [1024, D])
    w_o = din("w_o", [D, D])
    g2rep = din("g2rep", [128, D])
    gfrep = din("gfrep", [128, D])
    g2col = din("g2col", [128, 16])
    wr_d = din("wr", [D, 36])
    rb_d = din("rb", [128, 36])
    w_gate = din("w_gate", [NEXP, D, DE])
    w_up = din("w_up", [NEXP, D, DE])
    w_down = din("w_down", [NEXP, DE, D])
    tri_d = din("tri", [128, 128], BF16)
    eoff_d = din("eoff", [128, 32])
    eoffr_d = din("eoffr", [128, 512])
    tokid_d = din("tokid", [128, 32], I32)

    out_d = nc.dram_tensor("out", [OWN, D], F32, kind="ExternalOutput").ap()

    wib_s = dscr("wib_s", [D, IN_COLS], BF16)
    womb_s = dscr("womb_s", [1024, D], BF16)
    wodb_s = dscr("wodb_s", [1024, D], BF16)
    wob_s = dscr("wob_s", [D, D], BF16)
    kT_s = dscr("kT_s", [16, 128, S], BF16)
    v_s = dscr("v_s", [S, 2048], BF16)
    qT_s = dscr("qT_s", [16, 128, OWN], BF16)
    gT_s = dscr("gT_s", [32, 128, OWN], BF16)
    oT_s = dscr("oT_s", [16, 128, OWN], BF16)
    x1_s = dscr("x1_s", [OWN, D], F32)
    h2_s = dscr("h2_s", [OWN, D], BF16)
    rec_s = dscr("rec_s", [NSLOT + 128, 2], F32)
    y_s = dscr("y_s", [NSLOT + 128, D], F32)

    phases = dbg & {"A", "B", "C", "D"} or {"A", "B", "C", "D"}

    with ExitStack() as es:
        K = Ker(nc, es)

        def sb(es_, name, shape, dt):
            return es_.enter_context(nc.sbuf_tensor("sb_" + name, list(shape), dt))

        def ps(es_, name, shape, dt):
            return es_.enter_context(nc.psum_tensor("ps_" + name, list(shape), dt))

        A_COLS = [1024, 1536, 4096, 4608, 2048, 2560, 5120, 5632, 0, 512, 3072, 3584] + [6144 + 512 * s_ for s_ in range(8)]
        conv_list = [("in", c0) for c0 in A_COLS] + [(nm, cs_) for cs_ in range(4) for nm in ("om", "od")] + [("o", cs_) for cs_ in range(4)]
        conv_tok = {}
        conv_state = dict(ptr=0, hist=[])

        def conv_upto(n):
            while conv_state["ptr"] < min(n, len(conv_list)):
                kind, a_ = conv_list[conv_state["ptr"]]
                conv_state["ptr"] += 1
                if "chs" not in conv_state:
                    conv_state["chs"] = [K.chan() for _ in range(5)]
                ch = conv_state["chs"][(conv_state["ptr"] - 1) % 5]
                if len(conv_state["hist"]) >= 4:
                    K.wait("pool", [conv_state["hist"][-4]])
                if kind == "in":
                    tok = K.dma(ch, "pool", wib_s[:, a_:a_ + 512], w_in[:, a_:a_ + 512])
                elif kind == "om":
                    tok = K.dma(ch, "pool", womb_s[:, a_ * 512:(a_ + 1) * 512], w_om[:, a_ * 512:(a_ + 1) * 512])
                elif kind == "od":
                    tok = K.dma(ch, "pool", wodb_s[:, a_ * 512:(a_ + 1) * 512], w_od[:, a_ * 512:(a_ + 1) * 512])
                else:
                    tok = K.dma(ch, "pool", wob_s[:, a_ * 512:(a_ + 1) * 512], w_o[:, a_ * 512:(a_ + 1) * 512])
                conv_tok[(kind, a_)] = tok
                conv_state["hist"].append(tok)

        ident = Buf(sb(es, "ident", [128, 128], BF16), K.chan())
        ident.w(K.dma(ident.ch, "sp", ident.t[:], ident_d[:, :]))

        EPSC = Buf(sb(es, "EPSC", [128, 1], F32))
        EPSC.w(K.op("dve", lambda e: e.memset(EPSC.t[:], EPS)))

        if "A" in phases:
            with ExitStack() as ea:
                hT = [Buf(sb(ea, "hT%d" % i, [128, 16, 1024], BF16)) for i in range(2)]
                xin = [Buf(sb(ea, "xin%d" % i, [128, D], F32), K.chan()) for i in range(2)]
                hb = [Buf(sb(ea, "hb%d" % i, [128, D], BF16)) for i in range(2)]
                g1 = Buf(sb(ea, "g1", [128, D], F32), K.chan())
                bgs = Buf(sb(ea, "bgs", [128, 32], F32), K.chan())
                ss = Buf(sb(ea, "ss", [128, 8], F32))
                rs = Buf(sb(ea, "rs", [128, 8], F32))
                wsl = [Buf(sb(ea, "wsl%d" % i, [128, 16, 512], BF16), K.chan()) for i in range(3)]
                stg = [Buf(sb(ea, "stg%d" % i, [128, 512], BF16), K.chan()) for i in range(4)]
                pm = [Buf(ps(ea, "pm%d" % i, [128, 512], F32)) for i in range(6)]
                pt = [Buf(ps(ea, "pt%d" % i, [128, 4, 128], BF16)) for i in range(2)]

                g1.w(K.dma(g1.ch, "sp", g1.t[:], g1rep[:, :]))
                bgs.w(K.dma(bgs.ch, "sp", bgs.t[:], bg[:, :]))

                w_v = wib_s.rearrange("(c p) n -> p c n", p=128)
                st = dict(pm=0, stg=0, ev=0, slab=0, ptc=0)

                def load_slab(col0):
                    b = wsl[st["slab"] % 3]
                    conv_upto(st["slab"] + 4)
                    st["slab"] += 1
                    tok = K.dma(b.ch, "pool", b.t[:], w_v[:, :, col0:col0 + 512], deps=b.WR() + [conv_tok[("in", col0)]])
                    b.w(tok)
                    return b

                def evac(pbuf, dst_dram, func=None, bias=None, scale=None, ncols=512):
                    sgb = stg[st["stg"] % 4]
                    st["stg"] += 1
                    use_act = (func is not None) or (st["ev"] % 2 == 0)
                    st["ev"] += 1
                    deps = pbuf.RD() + sgb.WR()
                    if use_act:
                        kw = {}
                        if bias is not None:
                            kw["bias"] = bias
                        if scale is not None:
                            kw["scale"] = scale
                        tok = K.op("act", lambda e: e.activation(out=sgb.t[:, 0:ncols], in_=pbuf.t[:, 0:ncols],
                                                                  func=func or AF.Copy, **kw), deps)
                    else:
                        if scale is not None:
                            tok = K.op("dve", lambda e: e.tensor_scalar(out=sgb.t[:, 0:ncols], in0=pbuf.t[:, 0:ncols],
                                                                        scalar1=float(scale), scalar2=None, op0=ALU.mult), deps)
                        else:
                            tok = K.op("dve", lambda e: e.tensor_copy(out=sgb.t[:, 0:ncols], in_=pbuf.t[:, 0:ncols]), deps)
                    pbuf.r(tok)
                    sgb.w(tok)
                    stok = K.dma(sgb.ch, "sp", dst_dram, sgb.t[:, 0:ncols], deps=[tok], store=True)
                    sgb.r(stok)

                prep_first = {}

                def prep(g, tiles=range(8), part="both"):
                    H = hT[g % 2]
                    for t in tiles:
                      first = not prep_first.get(g, False)
                      if part in ("both", "pre"):
                          lt = 8 * g + t
                          xb = xin[t % 2]
                          hbb = hb[t % 2]
                          tok = K.dma(xb.ch, "sp", xb.t[:], xl[lt * 128:(lt + 1) * 128, :], deps=xb.WR())
                          xb.w(tok)
                          tok = K.op("act", lambda e: e.activation(out=hbb.t[:], in_=xb.t[:], func=AF.Square,
                                                                    accum_out=ss.t[:, t:t + 1]), xb.RD() + hbb.WR() + ss.WR())
                          xb.r(tok); hbb.w(tok); ss.w(tok, first=False)
                          tok = K.op("act", lambda e: e.activation(out=rs.t[:, t:t + 1], in_=ss.t[:, t:t + 1], func=AF.Sqrt,
                                                                    scale=1.0 / D, bias=EPS_AP[0]), [tok] + rs.WR())
                          rs.w(tok, first=False)
                          tok = K.op("dve", lambda e: e.reciprocal(out=rs.t[:, t:t + 1], in_=rs.t[:, t:t + 1]), [tok])
                          rs.w(tok, first=False)
                          tok = K.op("dve", lambda e: e.scalar_tensor_tensor(out=hbb.t[:], in0=xb.t[:], scalar=rs.t[:, t:t + 1],
                                                                             in1=g1.t[:], op0=ALU.mult, op1=ALU.mult),
                                     [tok] + hbb.WR() + g1.RD())
                          xb.r(tok); hbb.w(tok); rs.r(tok)
                      if part in ("both", "T"):
                          xb = xin[t % 2]
                          hbb = hb[t % 2]
                          for j in range(4):
                              pb_ = pt[st["ptc"] % 2]
                              st["ptc"] += 1
                              for c4 in range(4):
                                  c = 4 * j + c4
                                  tk = K.op("pe", lambda e: e.transpose(out=pb_.t[:, c4, :], in_=hbb.t[:, c * 128:(c + 1) * 128],
                                                                         identity=ident.t[:]),
                                            hbb.RD() + ident.RD() + (pb_.WR() if c4 == 0 else []))
                                  pb_.w(tk, first=(c4 == 0))
                              hbb.r(tk)
                              eng = "act" if (j % 2 == 0) else "dve"
                              dst = H.t[:, 4 * j:4 * j + 4, t * 128:(t + 1) * 128]
                              if eng == "act":
                                  tk2 = K.op("act", lambda e: e.activation(out=dst, in_=pb_.t[:], func=AF.Copy),
                                             pb_.RD() + (H.WR() if first else []))
                              else:
                                  tk2 = K.op("dve", lambda e: e.tensor_copy(out=dst, in_=pb_.t[:]),
                                             pb_.RD() + (H.WR() if first else []))
                              pb_.r(tk2)
                              H.w(tk2, first=first)
                              first = False
                              prep_first[g] = True

                EPS_AP = [None]
                epsb = Buf(sb(ea, "epsb", [128, 1], F32))
                tk = K.op("dve", lambda e: e.memset(epsb.t[:], EPS))
                epsb.w(tk)
                EPS_AP[0] = epsb.t[:, 0:1]
                K.wait("act", epsb.RD())

                def mm_group(out_ap, pairs, pbuf, extra_deps):
                    n = len(pairs)
                    tok = None
                    for i, (l, r_) in enumerate(pairs):
                        tok = K.op("pe", lambda e: e.matmul(out_ap, lhsT=l, rhs=r_, start=(i == 0), stop=(i == n - 1)),
                                   (extra_deps + pbuf.WR()) if i == 0 else [])
                    pbuf.w(tok)
                    return tok

                def next_pm():
                    b = pm[st["pm"] % 6]
                    st["pm"] += 1
                    return b

                NG = 8
                prep(0)
                for g in range(NG):
                    H = hT[g % 2]
                    for si, col0 in enumerate([1024, 1536, 4096, 4608]):
                        W = load_slab(col0)
                        for cb in range(4):
                            head = (0 if si < 2 else 8) + (si % 2) * 4 + cb
                            for half in range(2):
                                pbuf = next_pm()
                                tok = mm_group(pbuf.t[:, :],
                                               [(W.t[:, c, cb * 128:(cb + 1) * 128], H.t[:, c, half * 512:(half + 1) * 512])
                                                for c in range(16)], pbuf, W.RD() + H.RD())
                                evac(pbuf, kT_s[head, :, g * 1024 + half * 512: g * 1024 + (half + 1) * 512])
                        W.r(tok); H.r(tok)
                    for si, col0 in enumerate([2048, 2560, 5120, 5632]):
                        W = load_slab(col0)
                        vcol = (0 if si < 2 else 1024) + (si % 2) * 512
                        for t in range(8):
                            pbuf = next_pm()
                            tok = mm_group(pbuf.t[:, :],
                                           [(H.t[:, c, t * 128:(t + 1) * 128], W.t[:, c, :]) for c in range(16)],
                                           pbuf, W.RD() + H.RD())
                            lt = 8 * g + t
                            evac(pbuf, v_s[lt * 128:(lt + 1) * 128, vcol:vcol + 512])
                        W.r(tok); H.r(tok)
                    sched = []
                    if g + 1 < NG:
                        prep(g + 1, tiles=[0, 1], part="pre")
                        for t_ in range(8):
                            sched.append(("T", t_))
                            if t_ + 2 < 8:
                                sched.append(("pre", t_ + 2))
                    mmg = [0]

                    def tick():
                        mmg[0] += 1
                        if mmg[0] % 5 == 0 and sched:
                            kind, t_ = sched.pop(0)
                            prep(g + 1, tiles=[t_], part=kind)
                            if kind == "T" and sched and sched[0][0] == "pre":
                                kind, t_ = sched.pop(0)
                                prep(g + 1, tiles=[t_], part=kind)
                    def own_rhs(c):
                        return H.t[:, c, :].rearrange("p (a b) -> p a b", b=512)[:, :, 256:512]
                    for si, col0 in enumerate([0, 512, 3072, 3584]):
                        W = load_slab(col0)
                        for cb in range(4):
                            head = (0 if si < 2 else 8) + (si % 2) * 4 + cb
                            pbuf = next_pm()
                            tok = mm_group(pbuf.t[:, :].rearrange("p (a b) -> p a b", b=256),
                                           [(W.t[:, c, cb * 128:(cb + 1) * 128], own_rhs(c)) for c in range(16)],
                                           pbuf, W.RD() + H.RD())
                            evac(pbuf, qT_s[head, :, g * 512:(g + 1) * 512], scale=SCALE)
                            tick()
                        W.r(tok); H.r(tok)
                    for si in range(8):
                        W = load_slab(6144 + si * 512)
                        for cb in range(4):
                            j = si * 4 + cb
                            pbuf = next_pm()
                            tok = mm_group(pbuf.t[:, :].rearrange("p (a b) -> p a b", b=256),
                                           [(W.t[:, c, cb * 128:(cb + 1) * 128], own_rhs(c)) for c in range(16)],
                                           pbuf, W.RD() + H.RD() + bgs.RD())
                            evac(pbuf, gT_s[j, :, g * 512:(g + 1) * 512], func=AF.Sigmoid, bias=bgs.t[:, j:j + 1])
                            tick()
                        W.r(tok); H.r(tok)
                    while sched:
                        kind, t_ = sched.pop(0)
                        prep(g + 1, tiles=[t_], part=kind)
                endA = [(K.sem[e], K.cnt[e], e) for e in ["pe", "act", "dve"]]
                for e in ["pe", "act", "dve", "pool", "sp"]:
                    K.wait(e, endA)
                    K.barrier(e)

        if "B" in phases:
            with ExitStack() as eb:
                def cload(name, shape, dt, src_ap):
                    b = Buf(sb(eb, name, shape, dt), K.chan())
                    K.barrier("sp")
                    if len(shape) == 3:
                        dst = b.t[:].rearrange("p a b -> p (a b)")
                    elif len(shape) == 4:
                        dst = b.t[:].rearrange("p a b c -> p (a b c)")
                    else:
                        dst = b.t[:]
                    b.w(K.dma(b.ch, "sp", dst, src_ap))
                    return b
                ab = cload("ab", [128, NH, 64], F32, ab_d[:, :])
                ef = cload("ef", [128, NH, 2], F32, ef_d[:, :])
                bown = cload("bown", [128, NH, 256], F32, bown_d[:, :])
                pbq = cload("pbq", [128, 32 * 32], F32, pbq_d[:, :])
                pvq = cload("pvq", [128, 32 * 32], F32, pvq_d[:, :])
                esel = cload("esel", [128, 2, 32, 128], BF16, esel_d[:, :])
                selT = Buf(sb(eb, "selT", [128, OWN], BF16), K.chan())
                gm8 = Buf(sb(eb, "gm8", [128, 256], F32))
                mx8 = Buf(sb(eb, "mx8", [128, 8, 8], F32))
                sel8 = Buf(sb(eb, "sel8", [128, 256], F32))
                selb = Buf(sb(eb, "selb", [128, 8, 128], BF16))
                selb.w(K.op("dve", lambda e: e.memset(selb.t[:], 0.0)))
                selT.w(K.op("dve", lambda e: e.memset(selT.t[:], 0.0)))
                tot = [Buf(sb(eb, "tot%d" % i, [128, 129], F32)) for i in range(2)]
                valid = cload("valid", [128, NT], BF16, valid_d[:, :])
                kts = [Buf(sb(eb, "kts%d" % i, [128, S], BF16), K.chan()) for i in range(2)]
                vsb = [Buf(sb(eb, "vsb%d" % i, [128, NT, 129], BF16), K.chan()) for i in range(2)]
                qsb = [Buf(sb(eb, "qsb%d" % i, [128, OWN], BF16), K.chan()) for i in range(2)]
                rgb = [Buf(sb(eb, "rgb%d" % i, [128, 8, 2, 256], BF16), K.chan()) for i in range(2)]
                bownd = cload("bownd", [128, NH, 384], F32, bownd_d[:, :])
                abd = cload("abd", [128, NH, 17], F32, abd_d[:, :])
                efd = cload("efd", [128, NH, 2], F32, efd_d[:, :])
                ctl = cload("ctl", [128, 3, 512], BF16, ctl_d[:, :])
                lsd = cload("lsd", [128, 2, 128], BF16, lsd_d[:, :])
                sS = [Buf(sb(eb, "sS%d" % i, [128, 512], F32)) for i in range(3)]
                pP = [Buf(sb(eb, "pP%d" % i, [128, 512], BF16)) for i in range(4)]
                accs = [Buf(sb(eb, "accs%d" % i, [128, 2, 129], F32)) for i in range(2)]
                km = Buf(sb(eb, "km", [128, 32], F32))
                kmhl = Buf(sb(eb, "kmhl", [128, 64], BF16))
                rc = Buf(sb(eb, "rc", [128, 2], F32))
                ob = [Buf(sb(eb, "ob%d" % i, [128, 128], BF16)) for i in range(2)]
                ost = [Buf(sb(eb, "ost%d" % i, [128, 256], BF16), K.chan()) for i in range(2)]
                pS = [Buf(ps(eb, "pS%d" % i, [128, 512], F32)) for i in range(3)]
                pa = [Buf(ps(eb, "pa%d" % i, [128, 512], F32)) for i in range(4)]
                pgt_ = ps(eb, "pgt", [128, 512], F32)

                class _V:
                    pass
                ownp = Buf(_V()); ownp.t = pgt_[:, 0:258]
                pT = Buf(_V()); pT.t = pgt_[:, 258:386].bitcast(BF16).rearrange("p (u k) -> p u k", u=2)
                b7rd = {}

                def b7w():
                    return list(b7rd.values())

                def b7r(tok):
                    b7rd[tok[2]] = tok
                stB = dict(pS=0, sS=0, pP=0, ob=0, ost=0, pa=0)
                LAG = 2
                SLM = _slopes()[0].astype(np.float32).astype(np.float64)
                steps = []

                def nxt(lst, key):
                    b = lst[stB[key] % len(lst)]
                    stB[key] += 1
                    return b

                def load_head(Hh, vcol0, slot, dil_h=None):
                    kb, vb, qb = kts[slot], vsb[slot], qsb[slot]
                    K.barrier("sp")
                    kb.w(K.dma(kb.ch, "sp", kb.t[:, :], kT_s[Hh, :, :], deps=kb.WR()))
                    qb.w(K.dma(qb.ch, "sp", qb.t[:, :], qT_s[Hh, :, :], deps=qb.WR()))
                    wdeps = vb.WR()
                    vsrc = v_s[:, vcol0:vcol0 + 128].rearrange("(t p) d -> p t d", p=128)
                    tok = None
                    for q4 in range(4):
                        tok = K.dma(vb.ch, "sp", vb.t[:, q4 * 16:(q4 + 1) * 16, 0:128], vsrc[:, q4 * 16:(q4 + 1) * 16, :], deps=wdeps)
                    vb.w(tok)
                    tok2 = K.op("pool", lambda e: e.tensor_copy(out=vb.t[:, :, 128], in_=valid.t[:, :]), wdeps + valid.RD())
                    vb.w(tok2, first=False)
                    if dil_h is not None:
                        bb = rgb[dil_h % 2]
                        bb.w(K.dma(bb.ch, "sp", bb.t[:].rearrange("p a b c -> p (a b c)"), rg_d[dil_h, :, :], deps=bb.WR()))

                def add_finalize(src_fn, Hh, i):
                    stt = {}

                    def s2():
                        stt["o_st"] = nxt(ost, "ost")
                        stt["obb"] = []
                        prev = None
                        srcs = src_fn()
                        for u in range(2):
                            srcb, sap = srcs[u]
                            t1 = K.op("dve", lambda e: e.reciprocal(out=rc.t[:, u:u + 1], in_=sap[:, 128:129]), srcb.RD() + rc.WR() + [prev])
                            obb = nxt(ob, "ob")
                            t2 = K.op("dve", lambda e: e.tensor_scalar(out=obb.t[:, :], in0=sap[:, 0:128], scalar1=rc.t[:, u:u + 1],
                                                                        scalar2=None, op0=ALU.mult), [t1] + obb.WR())
                            obb.w(t2); srcb.r(t2); rc.w(t2)
                            stt["obb"].append(obb)
                            prev = t2

                    def s3():
                        o_st = stt["o_st"]
                        for u in range(2):
                            obb = stt["obb"][u]
                            t3 = K.op("pe", lambda e: e.transpose(out=pT.t[:, u, :], in_=obb.t[:, :], identity=ident.t[:]),
                                      obb.RD() + pT.WR() + ident.RD() + b7w())
                            obb.r(t3); pT.w(t3)
                            t4 = K.op("act", lambda e: e.activation(out=o_st.t[:, u * 128:(u + 1) * 128], in_=pT.t[:, u, :], func=AF.Copy),
                                      pT.RD() + (o_st.WR() if u == 0 else []))
                            pT.r(t4); b7r(t4); o_st.w(t4, first=(u == 0))
                        stok = K.dma(o_st.ch, "sp", oT_s[Hh, :, i * 256:(i + 1) * 256], o_st.t[:, :], deps=o_st.RD(), store=True)
                        o_st.r(stok)
                    steps.append(dict(s1=None, s2=s2, s3=s3))

                moba_pro = {}

                def moba_head(hm, slot):
                    kb, vb, qb = kts[slot], vsb[slot], qsb[slot]

                    def pro_k():
                        import os as _os
                        KD = _os.environ.get("KDBG", "")
                        if "0" in KD:
                            return
                        tok = K.op("dve", lambda e: e.tensor_reduce(out=km.t[:, :], in_=kb.t[:, :].rearrange("p (b k) -> p b k", k=256),
                                                                    axis=AX.X, op=ALU.add), kb.RD() + km.WR() + kmhl.WR())
                        km.w(tok); kb.r(tok)
                        tok = K.op("dve", lambda e: e.tensor_copy(out=kmhl.t[:, 0:32], in_=km.t[:, :]), [tok])
                        tok = K.op("dve", lambda e: e.tensor_tensor(out=kmhl.t[:, 32:64], in0=km.t[:, :], in1=kmhl.t[:, 0:32], op=ALU.subtract), [tok])
                        kmhl.w(tok); km.r(tok)
                        v_ = np.float32(SLM[hm] * 128.0)
                        hi_ = float(np.float32(v_.astype(ml_dtypes.bfloat16)))
                        lo_ = float(np.float32(np.float32(v_ - np.float32(hi_)).astype(ml_dtypes.bfloat16)))
                        tk = K.op("dve", lambda e: e.memset(selb.t[:, :, 32:33], hi_), selb.WR())
                        tk = K.op("dve", lambda e: e.memset(selb.t[:, :, 64:65], lo_), [tk])
                        selb.w(tk)
                        if "1" in KD:
                            return
                    def pro_a(g8):
                        pg = nxt(pS, "pS")
                        tok = None
                        for t8 in range(8):
                            qt = g8 * 8 + t8
                            tok = K.op("pe", lambda e: e.matmul(pg.t[:, t8 * 32:(t8 + 1) * 32], lhsT=qb.t[:, qt * 128:(qt + 1) * 128], rhs=kmhl.t[:, 0:32],
                                                                start=True, stop=False), (qb.RD() + kmhl.RD() + pg.WR()) if t8 == 0 else [])
                            tok = K.op("pe", lambda e: e.matmul(pg.t[:, t8 * 32:(t8 + 1) * 32], lhsT=qb.t[:, qt * 128:(qt + 1) * 128], rhs=kmhl.t[:, 32:64],
                                                                start=False, stop=True), [])
                        pg.w(tok); kmhl.r(tok); qb.r(tok)
                        d0 = K.op("dve", lambda e: e.tensor_tensor(out=gm8.t[:, :], in0=pg.t[:, 0:256], in1=pbq.t[:, g8 * 256:(g8 + 1) * 256], op=ALU.add),
                                  pg.RD() + pbq.RD() + gm8.WR())
                        pg.r(d0); gm8.w(d0)
                        dm = None
                        for t8 in range(8):
                            dm = K.op("dve", lambda e: e.max(out=mx8.t[:, t8, :], in_=gm8.t[:, t8 * 32:(t8 + 1) * 32]), [d0] + (mx8.WR() if t8 == 0 else []))
                        mx8.w(dm)
                        ds = None
                        for t8 in range(8):
                            qt = g8 * 8 + t8
                            ds = K.op("dve", lambda e: e.scalar_tensor_tensor(out=sel8.t[:, t8 * 32:(t8 + 1) * 32], in0=gm8.t[:, t8 * 32:(t8 + 1) * 32],
                                                                              scalar=mx8.t[:, t8, 2:3], in1=pvq.t[:, qt * 32:(qt + 1) * 32],
                                                                              op0=ALU.is_ge, op1=ALU.mult),
                                      [dm] + pvq.RD() + (sel8.WR() if t8 == 0 else []))
                        gm8.r(ds); mx8.r(ds)
                        ds = K.op("dve", lambda e: e.tensor_scalar(out=selb.t[:, :, 0:32], in0=sel8.t[:, :].rearrange("p (t k) -> p t k", k=32),
                                                                   scalar1=-1.0, scalar2=-NEG, op0=ALU.add, op1=ALU.mult),
                                  [ds] + selb.WR())
                        sel8.w(ds); sel8.r(ds); selb.w(ds, first=False)
                    def pro_b(g8):
                        for t2 in range(4):
                            tk = None
                            for u in range(2):
                                t8 = t2 * 2 + u
                                tk = K.op("pe", lambda e: e.transpose(out=pT.t[:, u, :], in_=selb.t[:, t8, :], identity=ident.t[:]),
                                          selb.RD() + ident.RD() + ((pT.WR() + b7w()) if u == 0 else []))
                            pT.w(tk)
                            q0 = (g8 * 8 + t2 * 2) * 128
                            first_ = (g8 == 0 and t2 == 0)
                            tk2 = K.op("act", lambda e: e.activation(out=selT.t[:, q0:q0 + 256], in_=pT.t[:, :, :].rearrange("p u k -> p (u k)"), func=AF.Copy),
                                       pT.RD() + (selT.WR() if first_ else []))
                            pT.r(tk2); b7r(tk2)
                            selT.w(tk2, first=first_)
                        selb.r(tk)
                    pro_steps = [dict(s1=pro_k, s2=None, s3=None)]
                    for g8_ in range(4):
                        pro_steps.append(dict(s1=(lambda g8_=g8_: pro_a(g8_)), s2=None, s3=None))
                        pro_steps.append(dict(s1=(lambda g8_=g8_: pro_b(g8_)), s2=None, s3=None))
                    moba_pro[hm] = pro_steps
                    import os as _os
                    KD = _os.environ.get("KDBG", "")
                    for i in range(16):
                        if "p" in KD:
                            break
                        ac = accs[i % 2]
                        lt0 = 4 * i + 2
                        qs = qb.t[:, i * 256:(i + 1) * 256]
                        pau = [pa[(i % 2) * 2], pa[(i % 2) * 2 + 1]]
                        nsel = 2 * i + 1
                        ctx = {}

                        def own1(i=i, lt0=lt0, qs=qs, ctx=ctx):
                            pSb = nxt(pS, "pS")
                            tok = K.op("pe", lambda e: e.matmul(pSb.t[:, 0:256], lhsT=kb.t[:, lt0 * 128:(lt0 + 1) * 128], rhs=qs,
                                                                start=True, stop=True), kb.RD() + qb.RD() + pSb.WR())
                            tok = K.op("pe", lambda e: e.matmul(pSb.t[:, 256:384], lhsT=kb.t[:, (lt0 + 1) * 128:(lt0 + 2) * 128],
                                                                rhs=qb.t[:, i * 256 + 128:(i + 1) * 256], start=True, stop=True), [])
                            pSb.w(tok); kb.r(tok); qb.r(tok)
                            s_ = nxt(sS, "sS")
                            t1 = K.op("dve", lambda e: e.tensor_tensor(out=s_.t[:, 0:256], in0=pSb.t[:, 0:256], in1=bown.t[:, hm, :], op=ALU.add),
                                      pSb.RD() + s_.WR() + bown.RD())
                            t1 = K.op("dve", lambda e: e.tensor_tensor(out=s_.t[:, 256:384], in0=pSb.t[:, 256:384], in1=bown.t[:, hm, 0:128],
                                                                       op=ALU.add), [t1])
                            s_.w(t1); pSb.r(t1)
                            p_ = nxt(pP, "pP")
                            t2 = K.op("act", lambda e: e.activation(out=p_.t[:, 0:384], in_=s_.t[:, 0:384], func=AF.Exp), s_.RD() + p_.WR())
                            p_.w(t2); s_.r(t2)
                            ctx["p"] = p_

                        def own2(lt0=lt0, ac=ac, pau=pau, ctx=ctx):
                            p_ = ctx["p"]
                            t3 = K.op("pe", lambda e: e.matmul(ownp.t[:, 0:129], lhsT=p_.t[:, 0:128], rhs=vb.t[:, lt0, :], start=True, stop=True),
                                      p_.RD() + vb.RD() + ownp.WR() + b7w())
                            t3b = K.op("pe", lambda e: e.matmul(ownp.t[:, 129:258], lhsT=p_.t[:, 128:256], rhs=vb.t[:, lt0, :], start=True, stop=False), [])
                            t3b = K.op("pe", lambda e: e.matmul(ownp.t[:, 129:258], lhsT=p_.t[:, 256:384], rhs=vb.t[:, lt0 + 1, :], start=False, stop=True), [])
                            ownp.w(t3b); p_.r(t3b); vb.r(t3b)
                            a1 = K.op("act", lambda e: e.activation(out=ac.t[:, :, :].rearrange("p u k -> p (u k)"), in_=ownp.t[:, 0:258], func=AF.Copy),
                                      [t3b] + ac.WR())
                            ac.w(a1); ownp.r(a1); b7r(a1)
                        steps.append(dict(s1=own1, s2=own2, s3=None))

                        for lbp in range(nsel):
                            if "o" in KD:
                                break
                            cx = {}

                            def sel1(i=i, lbp=lbp, qs=qs, cx=cx):
                                pSb = nxt(pS, "pS")
                                tok = None
                                for kk in range(2):
                                    kt = 2 * lbp + kk
                                    tok = K.op("pe", lambda e: e.matmul(pSb.t[:, kk * 256:(kk + 1) * 256], lhsT=kb.t[:, kt * 128:(kt + 1) * 128], rhs=qs,
                                                                        start=True, stop=False),
                                               (kb.RD() + qb.RD() + pSb.WR() + selT.RD() + esel.RD()) if kk == 0 else [])
                                    tok = K.op("pe", lambda e: e.matmul(pSb.t[:, kk * 256:(kk + 1) * 256], lhsT=esel.t[:, kk, lbp, :],
                                                                        rhs=selT.t[:, i * 256:(i + 1) * 256], start=False, stop=True), [])
                                pSb.w(tok); kb.r(tok); qb.r(tok); selT.r(tok)
                                p_ = nxt(pP, "pP")
                                m0 = 4 * i + 2 - 2 * lbp
                                t2 = K.op("act", lambda e: e.activation(out=p_.t[:, 0:512], in_=pSb.t[:, 0:512], func=AF.Exp, bias=ab.t[:, hm, m0:m0 + 1], scale=1.0),
                                          pSb.RD() + ab.RD() + p_.WR())
                                p_.w(t2); pSb.r(t2)
                                cx["p"] = p_

                            def sel2(lbp=lbp, nsel=nsel, pau=pau, cx=cx):
                                p_ = cx["p"]
                                t3 = None
                                for u in range(2):
                                    for kk in range(2):
                                        first = (lbp == 0 and kk == 0)
                                        last = (lbp == nsel - 1 and kk == 1)
                                        t3 = K.op("pe", lambda e: e.matmul(pau[u].t[:, 0:129], lhsT=p_.t[:, kk * 256 + u * 128:kk * 256 + (u + 1) * 128],
                                                                           rhs=vb.t[:, 2 * lbp + kk, :], start=first, stop=last),
                                                  (p_.RD() + vb.RD() + (pau[0].WR() + pau[1].WR() if lbp == 0 else [])) if (u == 0 and kk == 0) else [])
                                    if lbp == nsel - 1:
                                        pau[u].w(t3)
                                p_.r(t3); vb.r(t3)
                            steps.append(dict(s1=sel1, s2=sel2, s3=None))

                        def fin_src(i=i, ac=ac, pau=pau, hm=hm):
                            res = []
                            prev = None
                            for u in range(2):
                                tt_ = tot[u]
                                prev = K.op("dve", lambda e: e.scalar_tensor_tensor(out=tt_.t[:, :], in0=pau[u].t[:, 0:129], scalar=ef.t[:, hm, u:u + 1],
                                                                                    in1=ac.t[:, u, :], op0=ALU.mult, op1=ALU.add),
                                            pau[u].RD() + ac.RD() + ef.RD() + tt_.WR() + [prev])
                                tt_.w(prev); pau[u].r(prev); ac.r(prev)
                                res.append((tt_, tt_.t[:, :]))
                            return res
                        if "o" not in KD:
                            add_finalize(fin_src, hm, i)

                def dil_head(hd, slot):
                    kb, vb, qb = kts[slot], vsb[slot], qsb[slot]
                    rg = rgb[hd % 2]
                    for i in range(16):
                        ac = accs[i % 2]
                        lt0 = 4 * i + 2
                        qs = qb.t[:, i * 256:(i + 1) * 256]
                        pau = [pa[(i % 2) * 2], pa[(i % 2) * 2 + 1]]
                        npair = min(8, lt0 // 2)
                        ctx = {}

                        def own1(i=i, lt0=lt0, qs=qs, ctx=ctx):
                            pSb = nxt(pS, "pS")
                            tok = K.op("pe", lambda e: e.matmul(pSb.t[:, 0:256], lhsT=kb.t[:, lt0 * 128:(lt0 + 1) * 128], rhs=qs,
                                                                start=True, stop=True), kb.RD() + qb.RD() + pSb.WR())
                            tok = K.op("pe", lambda e: e.matmul(pSb.t[:, 256:384], lhsT=kb.t[:, (lt0 + 1) * 128:(lt0 + 2) * 128],
                                                                rhs=qb.t[:, i * 256 + 128:(i + 1) * 256], start=True, stop=True), [])
                            pSb.w(tok); kb.r(tok); qb.r(tok)
                            s_ = nxt(sS, "sS")
                            t1 = K.op("dve", lambda e: e.tensor_tensor(out=s_.t[:, 0:384], in0=pSb.t[:, 0:384], in1=bownd.t[:, hd, :], op=ALU.add),
                                      pSb.RD() + s_.WR() + bownd.RD())
                            s_.w(t1); pSb.r(t1)
                            p_ = nxt(pP, "pP")
                            t2 = K.op("act", lambda e: e.activation(out=p_.t[:, 0:384], in_=s_.t[:, 0:384], func=AF.Exp), s_.RD() + p_.WR())
                            p_.w(t2); s_.r(t2)
                            ctx["p"] = p_

                        def own2(lt0=lt0, ac=ac, ctx=ctx):
                            p_ = ctx["p"]
                            t3 = K.op("pe", lambda e: e.matmul(ownp.t[:, 0:129], lhsT=p_.t[:, 0:128], rhs=vb.t[:, lt0, :], start=True, stop=True),
                                      p_.RD() + vb.RD() + ownp.WR() + b7w())
                            t3b = K.op("pe", lambda e: e.matmul(ownp.t[:, 129:258], lhsT=p_.t[:, 128:256], rhs=vb.t[:, lt0, :], start=True, stop=False), [])
                            t3b = K.op("pe", lambda e: e.matmul(ownp.t[:, 129:258], lhsT=p_.t[:, 256:384], rhs=vb.t[:, lt0 + 1, :], start=False, stop=True), [])
                            ownp.w(t3b); p_.r(t3b); vb.r(t3b)
                            a1 = K.op("act", lambda e: e.activation(out=ac.t[:, :, :].rearrange("p u k -> p (u k)"), in_=ownp.t[:, 0:258], func=AF.Copy),
                                      [t3b] + ac.WR())
                            ac.w(a1); ownp.r(a1); b7r(a1)
                        steps.append(dict(s1=own1, s2=own2, s3=None))

                        for pp in range(npair - 1, -1, -1):
                            cx = {}
                            kt0 = lt0 - (2 * pp + 2)

                            def r1(i=i, pp=pp, kt0=kt0, qs=qs, cx=cx):
                                pSb = nxt(pS, "pS")
                                tok = None
                                for kk in range(2):
                                    kt = kt0 + kk
                                    tok = K.op("pe", lambda e: e.matmul(pSb.t[:, kk * 256:(kk + 1) * 256], lhsT=kb.t[:, kt * 128:(kt + 1) * 128], rhs=qs,
                                                                        start=True, stop=False),
                                               (kb.RD() + qb.RD() + pSb.WR() + rg.RD() + lsd.RD()) if kk == 0 else [])
                                    tok = K.op("pe", lambda e: e.matmul(pSb.t[:, kk * 256:(kk + 1) * 256], lhsT=lsd.t[:, kk, :],
                                                                        rhs=rg.t[:, pp, kk, :], start=False, stop=True), [])
                                pSb.w(tok); kb.r(tok); qb.r(tok); rg.r(tok)
                                p_ = nxt(pP, "pP")
                                dd_far = 2 * pp + 2
                                t2 = K.op("act", lambda e: e.activation(out=p_.t[:, 0:512], in_=pSb.t[:, 0:512], func=AF.Exp, bias=abd.t[:, hd, dd_far:dd_far + 1], scale=1.0),
                                          pSb.RD() + abd.RD() + p_.WR())
                                p_.w(t2); pSb.r(t2)
                                if pp in (0, 1, 7):
                                    ci = {0: 0, 1: 1, 7: 2}[pp]
                                    eng = "dve" if (stB["pa"] % 2 == 0) else "pool"
                                    stB["pa"] += 1
                                    t2 = K.op(eng, lambda e: e.tensor_tensor(out=p_.t[:, 0:512], in0=p_.t[:, 0:512], in1=ctl.t[:, ci, :], op=ALU.mult),
                                              [t2] + ctl.RD())
                                    p_.w(t2)
                                cx["p"] = p_

                            def r2(pp=pp, kt0=kt0, npair=npair, pau=pau, cx=cx):
                                p_ = cx["p"]
                                t3 = None
                                for u in range(2):
                                    for kk in range(2):
                                        first = (pp == npair - 1 and kk == 0)
                                        last = (pp == 0 and kk == 1)
                                        t3 = K.op("pe", lambda e: e.matmul(pau[u].t[:, 0:129], lhsT=p_.t[:, kk * 256 + u * 128:kk * 256 + (u + 1) * 128],
                                                                           rhs=vb.t[:, kt0 + kk, :], start=first, stop=last),
                                                  (p_.RD() + vb.RD() + (pau[0].WR() + pau[1].WR() if pp == npair - 1 else [])) if (u == 0 and kk == 0) else [])
                                    if pp == 0:
                                        pau[u].w(t3)
                                p_.r(t3); vb.r(t3)
                            steps.append(dict(s1=r1, s2=r2, s3=None))

                        def fin_src(i=i, ac=ac, pau=pau, hd=hd):
                            res = []
                            prev = None
                            for u in range(2):
                                tt_ = tot[u]
                                prev = K.op("dve", lambda e: e.scalar_tensor_tensor(out=tt_.t[:, :], in0=pau[u].t[:, 0:129], scalar=efd.t[:, hd, u:u + 1],
                                                                                    in1=ac.t[:, u, :], op0=ALU.mult, op1=ALU.add),
                                            pau[u].RD() + ac.RD() + efd.RD() + tt_.WR() + [prev])
                                tt_.w(prev); pau[u].r(prev); ac.r(prev)
                                res.append((tt_, tt_.t[:, :]))
                            return res
                        add_finalize(fin_src, 8 + hd, i)

                order = []
                for h in range(NH):
                    order.append(("m", h))
                    order.append(("d", h))
                if "Bm" in dbg:
                    order = [("m", 0), ("d", 0)]
                if "Bm1" in dbg:
                    order = [("m", 0)]
                if "Bd1" in dbg:
                    import os as _os2
                    order = [("d", int(_os2.environ.get("KDH", "1")))]

                def mk_load(idx):
                    kind, h = order[idx]
                    if kind == "m":
                        return lambda: load_head(h, h * 128, idx % 2)
                    return lambda: load_head(8 + h, 1024 + h * 128, idx % 2, dil_h=h)
                mk_load(0)()
                if len(order) > 1:
                    mk_load(1)()
                n0_prev = 0
                for idx, (kind, h) in enumerate(order):
                    n0 = len(steps)
                    if kind == "m":
                        moba_head(h, idx % 2)
                        ps_ = moba_pro[h]
                        if idx >= 2 and n0 - n0_prev > 100:
                            offs = [30, 34, 41, 44, 51, 54, 61, 64, 71]
                            for st_p, of_ in zip(ps_, offs):
                                steps.insert(n0_prev + of_, st_p)
                            n0 += len(ps_)
                        else:
                            for k_p, st_p in enumerate(ps_):
                                steps.insert(n0 + k_p, st_p)
                    else:
                        dil_head(h, idx % 2)
                    if idx >= 1 and idx + 1 < len(order):
                        steps.insert(n0 + 2 * LAG + 3, dict(s1=mk_load(idx + 1), s2=None, s3=None))
                    n0_prev = n0
                ns = len(steps)
                for t in range(ns + 2 * LAG):
                    if t < ns and steps[t]["s1"]:
                        steps[t]["s1"]()
                    if 0 <= t - LAG < ns and steps[t - LAG]["s2"]:
                        steps[t - LAG]["s2"]()
                    if 0 <= t - 2 * LAG < ns and steps[t - 2 * LAG]["s3"]:
                        steps[t - 2 * LAG]["s3"]()
                endB = [(K.sem[e], K.cnt[e], e) for e in ["pe", "act", "dve", "pool"]]
                for e in ["pe", "act", "dve", "pool", "sp"]:
                    K.wait(e, endB)
                    K.barrier(e)

        if "C" in phases:
            with ExitStack() as ec:
                def cload2(name, shape, dt, src_ap, q="sp"):
                    b = Buf(sb(ec, name, shape, dt), K.chan())
                    b.w(K.dma(b.ch, q, b.t[:], src_ap))
                    return b
                K.barrier("sp"); K.barrier("pool")
                g2r = cload2("g2r", [128, D], F32, g2rep[:, :])
                rbb = cload2("rbb", [128, 36], F32, rb_d[:, :])
                g2c = cload2("g2c", [128, 16], F32, g2col[:, :])
                wrb = cload2("wrb", [128, 16, 36], F32, wr_d.rearrange("(c p) n -> p c n", p=128))
                identf = cload2("identf", [128, 128], F32, identf_d[:, :])
                A1all = Buf(sb(ec, "A1all", [128, 32, 32], F32))
                A2all = Buf(sb(ec, "A2all", [128, 32, 32], F32))
                Aall = Buf(sb(ec, "Aall", [128, 32, 32], BF16))
                wts = Buf(sb(ec, "wts", [128, 32, 2], F32))
                rstd2 = Buf(sb(ec, "rstd2", [128, 32], F32))
                dv = None
                for c in range(16):
                    dv = K.op("dve", lambda e: e.tensor_scalar(out=wrb.t[:, c, :], in0=wrb.t[:, c, :], scalar1=g2c.t[:, c:c + 1], scalar2=None,
                                                               op0=ALU.mult), wrb.RD() + g2c.RD() + [dv])
                wrb.w(dv, first=False)

                with ExitStack() as ecc:
                    oTb = Buf(sb(ecc, "oTb", [128, 16, 512], BF16), K.chan())
                    gsl = [Buf(sb(ecc, "gsl%d" % i, [128, 8, 512], BF16), K.chan()) for i in range(2)]
                    wms = [Buf(sb(ecc, "wms%d" % i, [128, 8, 512], BF16), K.chan()) for i in range(4)]
                    wos = [Buf(sb(ecc, "wos%d" % i, [128, 16, 512], BF16), K.chan()) for i in range(2)]
                    zT = Buf(sb(ecc, "zT", [128, 16, 512], BF16))
                    x1b = [Buf(sb(ecc, "x1b%d" % i, [128, D], F32), K.chan()) for i in range(4)]
                    x1st = [K.chan() for i in range(4)]
                    t1b = [Buf(sb(ecc, "t1b%d" % i, [128, 512], F32)) for i in range(2)]
                    t2b = [Buf(sb(ecc, "t2b%d" % i, [128, 512], F32)) for i in range(2)]
                    h2b = Buf(sb(ecc, "h2b", [128, D], BF16), K.chan())
                    x1T = Buf(sb(ecc, "x1T", [128, 16, 128], F32))
                    lgs = [Buf(sb(ecc, "lg%d" % i, [128, 36], F32)) for i in range(4)]
                    sm_ = Buf(sb(ecc, "sm_", [128, 64], F32))
                    py = [Buf(ps(ecc, "py%d" % i, [128, 512], F32)) for i in range(4)]
                    pw = [Buf(ps(ecc, "pw%d" % i, [128, 512], F32)) for i in range(2)]
                    ptf = Buf(ps(ecc, "ptf", [128, 4, 128], F32))
                    pl = Buf(ps(ecc, "pl", [128, 512], F32))
                    stC = dict(wms=0, wos=0, gsl=0, py=0, pw=0, t=0)
                    conv_upto(len(conv_list))
                    wom_v = womb_s.rearrange("(h p) n -> p h n", p=128)
                    wod_v = wodb_s.rearrange("(h p) n -> p h n", p=128)
                    wo_v = wob_s.rearrange("(c p) n -> p c n", p=128)

                    def nxc(lst, key):
                        b = lst[stC[key] % len(lst)]
                        stC[key] += 1
                        return b

                    def c_xload(tg):
                        tsl = slice(tg * 512, (tg + 1) * 512)
                        for tt in range(4):
                            ot = tg * 4 + tt
                            lt = 4 * (ot // 2) + 2 + (ot % 2)
                            xb = x1b[tt]
                            xb.w(K.dma(xb.ch, "sp", xb.t[:], xl[lt * 128:(lt + 1) * 128, :], deps=xb.WR()))

                    def c_ystage(tg):
                        tsl = slice(tg * 512, (tg + 1) * 512)
                        oTb.w(K.dma(oTb.ch, "sp", oTb.t[:], oT_s[:, :, tsl].rearrange("h p n -> p h n"), deps=oTb.WR()))
                        tokz = None
                        def y_loads(cs):
                            csl = slice(cs * 512, (cs + 1) * 512)
                            wm = nxc(wms, "wms")
                            wm.w(K.dma(wm.ch, "pool", wm.t[:], wom_v[:, :, csl], deps=wm.WR() + [conv_tok[("om", cs)]]))
                            wd_ = nxc(wms, "wms")
                            wd_.w(K.dma(wd_.ch, "pool", wd_.t[:], wod_v[:, :, csl], deps=wd_.WR() + [conv_tok[("od", cs)]]))
                            gs = nxc(gsl, "gsl")
                            wdeps = gs.WR()
                            K.dma(gs.ch, "sp", gs.t[:, 0:4, :], gT_s[cs * 4:cs * 4 + 4, :, tsl].rearrange("j p n -> p j n"), deps=wdeps)
                            gs.w(K.dma(gs.ch, "sp", gs.t[:, 4:8, :], gT_s[16 + cs * 4:16 + cs * 4 + 4, :, tsl].rearrange("j p n -> p j n"), deps=wdeps))
                            return wm, wd_, gs
                        nxt_l = y_loads(0)
                        for cs in range(4):
                            csl = slice(cs * 512, (cs + 1) * 512)
                            wm, wd_, gs = nxt_l
                            if cs + 1 < 4:
                                nxt_l = y_loads(cs + 1)
                            for cb in range(4):
                                j = cs * 4 + cb
                                pym = nxc(py, "py")
                                pyd = nxc(py, "py")
                                tok = None
                                for hh in range(8):
                                    tok = K.op("pe", lambda e: e.matmul(pym.t[:, :], lhsT=wm.t[:, hh, cb * 128:(cb + 1) * 128], rhs=oTb.t[:, hh, :],
                                                                        start=(hh == 0), stop=(hh == 7)),
                                               (wm.RD() + oTb.RD() + pym.WR()) if hh == 0 else [])
                                pym.w(tok)
                                for hh in range(8):
                                    tok = K.op("pe", lambda e: e.matmul(pyd.t[:, :], lhsT=wd_.t[:, hh, cb * 128:(cb + 1) * 128], rhs=oTb.t[:, 8 + hh, :],
                                                                        start=(hh == 0), stop=(hh == 7)),
                                               (wd_.RD() + pyd.WR()) if hh == 0 else [])
                                pyd.w(tok)
                                wm.r(tok); wd_.r(tok); oTb.r(tok)
                                ta = t1b[stC["t"] % 2]
                                tb = t2b[stC["t"] % 2]
                                stC["t"] += 1
                                d1 = K.op("dve", lambda e: e.tensor_tensor(out=ta.t[:, :], in0=pym.t[:, :], in1=gs.t[:, cb, :], op=ALU.mult),
                                          pym.RD() + gs.RD() + ta.WR())
                                ta.w(d1); pym.r(d1)
                                d2 = K.op("dve", lambda e: e.tensor_tensor(out=tb.t[:, :], in0=pyd.t[:, :], in1=gs.t[:, 4 + cb, :], op=ALU.mult),
                                          pyd.RD() + tb.WR())
                                tb.w(d2); pyd.r(d2); gs.r(d2)
                                tokz = K.op("pool", lambda e: e.tensor_tensor(out=zT.t[:, j, :], in0=ta.t[:, :], in1=tb.t[:, :], op=ALU.add),
                                            ta.RD() + tb.RD() + (zT.WR() if j == 0 else []))
                                ta.r(tokz); tb.r(tokz)
                                zT.w(tokz, first=(j == 0))

                    def c_wostage(tg):
                        tsl = slice(tg * 512, (tg + 1) * 512)
                        for cs in range(4):
                            csl = slice(cs * 512, (cs + 1) * 512)
                            wo_ = nxc(wos, "wos")
                            wo_.w(K.dma(wo_.ch, "pool", wo_.t[:], wo_v[:, :, csl], deps=wo_.WR() + [conv_tok[("o", cs)]]))
                            for tt in range(4):
                                pwb = nxc(pw, "pw")
                                tok = None
                                for c in range(16):
                                    tok = K.op("pe", lambda e: e.matmul(pwb.t[:, :], lhsT=zT.t[:, c, tt * 128:(tt + 1) * 128], rhs=wo_.t[:, c, :],
                                                                        start=(c == 0), stop=(c == 15)),
                                               (zT.RD() + wo_.RD() + pwb.WR()) if c == 0 else [])
                                pwb.w(tok)
                                xb = x1b[tt]
                                d1 = K.op("dve", lambda e: e.tensor_tensor(out=xb.t[:, csl], in0=pwb.t[:, :], in1=xb.t[:, csl], op=ALU.add),
                                          pwb.RD() + xb.WR())
                                pwb.r(d1); xb.w(d1, first=False)
                            wo_.r(tok); zT.r(tok)

                    def c_tail(tg):
                        tsl = slice(tg * 512, (tg + 1) * 512)
                        for tt in range(4):
                            n = tg * 4 + tt
                            xb = x1b[tt]
                            rows = slice(n * 128, (n + 1) * 128)
                            stok = K.dma(x1st[tt], "sp", x1_s[rows, :], xb.t[:], deps=xb.RD(), store=True)
                            xb.r(stok)
                            a1 = K.op("act", lambda e: e.activation(out=h2b.t[:], in_=xb.t[:], func=AF.Square, accum_out=sm_.t[:, 0:1]),
                                      xb.RD() + h2b.WR() + sm_.WR())
                            xb.r(a1)
                            a1 = K.op("act", lambda e: e.activation(out=rstd2.t[:, n:n + 1], in_=sm_.t[:, 0:1], func=AF.Sqrt, scale=1.0 / D, bias=EPSC.t[:, 0:1]),
                                      [a1] + EPSC.RD())
                            dv = K.op("dve", lambda e: e.reciprocal(out=rstd2.t[:, n:n + 1], in_=rstd2.t[:, n:n + 1]), [a1] + sm_.WR())
                            dv = K.op("dve", lambda e: e.scalar_tensor_tensor(out=h2b.t[:], in0=xb.t[:], scalar=rstd2.t[:, n:n + 1], in1=g2r.t[:],
                                                                              op0=ALU.mult, op1=ALU.mult), [dv] + g2r.RD())
                            h2b.w(dv); xb.r(dv)
                            stok = K.dma(h2b.ch, "sp", h2_s[rows, :], h2b.t[:], deps=[dv], store=True)
                            h2b.r(stok)
                            for j4 in range(4):
                                tk = None
                                for c4 in range(4):
                                    c = 4 * j4 + c4
                                    tk = K.op("pe", lambda e: e.transpose(out=ptf.t[:, c4, :], in_=xb.t[:, c * 128:(c + 1) * 128], identity=identf.t[:]),
                                              xb.RD() + identf.RD() + (ptf.WR() if c4 == 0 else []))
                                ptf.w(tk)
                                tk2 = K.op("act", lambda e: e.activation(out=x1T.t[:, 4 * j4:4 * j4 + 4, :], in_=ptf.t[:], func=AF.Copy),
                                           ptf.RD() + (x1T.WR() if j4 == 0 else []))
                                ptf.r(tk2)
                                x1T.w(tk2, first=(j4 == 0))
                            xb.r(tk)
                            tok = None
                            for c in range(16):
                                tok = K.op("pe", lambda e: e.matmul(pl.t[:, 0:36], lhsT=x1T.t[:, c, :], rhs=wrb.t[:, c, :], start=(c == 0), stop=(c == 15)),
                                           (x1T.RD() + wrb.RD() + pl.WR()) if c == 0 else [])
                            pl.w(tok); x1T.r(tok)
                            dv = K.op("dve", lambda e: e.scalar_tensor_tensor(out=lgs[tt].t[:, :], in0=pl.t[:, 0:36], scalar=rstd2.t[:, n:n + 1], in1=rbb.t[:, :],
                                                                              op0=ALU.mult, op1=ALU.add), pl.RD() + rbb.RD() + [dv])
                            pl.r(dv)
                        dv_a = dv
                        for tt in range(4):
                            n = tg * 4 + tt
                            if tt == 0:
                                dv = dv_a
                            S_ = sm_.t
                            dv = K.op("dve", lambda e: e.tensor_reduce(out=S_[:, 1:2], in_=lgs[tt].t[:, 0:4], axis=AX.X, op=ALU.max), [dv])
                            dv = K.op("dve", lambda e: e.tensor_scalar(out=S_[:, 2:6], in0=lgs[tt].t[:, 0:4], scalar1=S_[:, 1:2], scalar2=None, op0=ALU.is_ge), [dv])
                            dv = K.op("dve", lambda e: e.tensor_scalar(out=S_[:, 6:7], in0=S_[:, 1:2], scalar1=-1.0, scalar2=None, op0=ALU.mult), [dv])
                            a1 = K.op("act", lambda e: e.activation(out=S_[:, 8:12], in_=lgs[tt].t[:, 0:4], func=AF.Exp, bias=S_[:, 6:7], scale=1.0,
                                                                     accum_out=S_[:, 7:8]), [dv])
                            dv = K.op("dve", lambda e: e.reciprocal(out=S_[:, 7:8], in_=S_[:, 7:8]), [a1])
                            dv = K.op("dve", lambda e: e.tensor_scalar(out=S_[:, 16:24], in0=lgs[tt].t[:, 4:12], scalar1=S_[:, 2:3], scalar2=None, op0=ALU.mult), [dv])
                            for g_ in range(1, 4):
                                dv = K.op("dve", lambda e: e.scalar_tensor_tensor(out=S_[:, 16:24], in0=lgs[tt].t[:, 4 + 8 * g_:12 + 8 * g_], scalar=S_[:, 2 + g_:3 + g_],
                                                                                  in1=S_[:, 16:24], op0=ALU.mult, op1=ALU.add), [dv])
                            dv = K.op("dve", lambda e: e.max(out=S_[:, 24:32], in_=S_[:, 16:24]), [dv])
                            for g_ in range(4):
                                dv = K.op("dve", lambda e: e.tensor_scalar(out=A1all.t[:, n, 8 * g_:8 * g_ + 8], in0=lgs[tt].t[:, 4 + 8 * g_:12 + 8 * g_],
                                                                           scalar1=S_[:, 24:25], scalar2=S_[:, 2 + g_:3 + g_], op0=ALU.is_equal, op1=ALU.mult), [dv])
                                dv = K.op("dve", lambda e: e.tensor_scalar(out=A2all.t[:, n, 8 * g_:8 * g_ + 8], in0=lgs[tt].t[:, 4 + 8 * g_:12 + 8 * g_],
                                                                           scalar1=S_[:, 25:26], scalar2=S_[:, 2 + g_:3 + g_], op0=ALU.is_equal, op1=ALU.mult), [dv])
                            dv = K.op("dve", lambda e: e.tensor_tensor(out=Aall.t[:, n, :], in0=A1all.t[:, n, :], in1=A2all.t[:, n, :], op=ALU.add), [dv])
                            dv = K.op("dve", lambda e: e.tensor_tensor(out=S_[:, 32:33], in0=S_[:, 25:26], in1=S_[:, 24:25], op=ALU.subtract), [dv])
                            a1 = K.op("act", lambda e: e.activation(out=S_[:, 33:34], in_=S_[:, 32:33], func=AF.Exp), [dv])
                            dv = K.op("dve", lambda e: e.tensor_scalar(out=S_[:, 34:35], in0=S_[:, 33:34], scalar1=1.0, scalar2=None, op0=ALU.add), [a1])
                            dv = K.op("dve", lambda e: e.reciprocal(out=S_[:, 34:35], in_=S_[:, 34:35]), [dv])
                            dv = K.op("dve", lambda e: e.tensor_tensor(out=wts.t[:, n, 0:1], in0=S_[:, 34:35], in1=S_[:, 7:8], op=ALU.mult), [dv])
                            dv = K.op("dve", lambda e: e.tensor_tensor(out=wts.t[:, n, 1:2], in0=wts.t[:, n, 0:1], in1=S_[:, 33:34], op=ALU.mult), [dv])
                            sm_.w(dv); sm_.r(dv)
                            for b_ in (A1all, A2all, Aall, wts, rstd2):
                                b_.w(dv, first=False)

                    c_xload(0)
                    c_ystage(0)
                    c_wostage(0)
                    for tg in range(1, 8):
                        c_ystage(tg)
                        c_tail(tg - 1)
                        c_xload(tg)
                        c_wostage(tg)
                    c_tail(7)
                    endC = [(K.sem[e], K.cnt[e], e) for e in ["pe", "act", "dve", "pool"]]
                    for e in ["pe", "act", "dve", "pool", "sp"]:
                        K.wait(e, endC)
                        K.barrier(e)

                if "D" in phases:
                    with ExitStack() as ed:
                        tri = cload2("tri", [128, 128], BF16, tri_d[:, :])
                        eoff = cload2("eoff", [128, 32], F32, eoff_d[:, :])
                        tokid = cload2("tokid", [128, 32], I32, tokid_d[:, :])
                        gfr = cload2("gfr", [128, D], F32, gfrep[:, :])
                        eoffr = cload2("eoffr", [128, 512], F32, eoffr_d[:, :])
                        ones = Buf(sb(ed, "ones", [128, 128], BF16))
                        zer = Buf(sb(ed, "zer", [128, D], F32))
                        dsti = Buf(sb(ed, "dsti", [128, 32, 2], I32))
                        recs = Buf(sb(ed, "recs", [128, 32, 2, 2], F32))
                        rk = Buf(sb(ed, "rk", [128, 512], F32))
                        okm = Buf(sb(ed, "okm", [128, 512], F32))
                        tm = Buf(sb(ed, "tm", [128, 512], F32))
                        s4 = Buf(sb(ed, "s4", [128, 4, 16], F32))
                        pr = [Buf(ps(ed, "pr%d" % i, [128, 512], F32)) for i in range(2)]
                        dv = K.op("dve", lambda e: e.memset(ones.t[:], 1.0))
                        ones.w(dv)
                        dv = K.op("dve", lambda e: e.memset(zer.t[:], 0.0), [dv])
                        zer.w(dv)
                        chz = K.chan()
                        z1 = K.dma(chz, "sp", rec_s.rearrange("(p a) two -> p (a two)", p=128), zer.t[:, 0:(NSLOT + 128) // 128 * 2], deps=zer.RD(), store=True)
                        z2 = K.dma(chz, "sp", y_s[NSLOT:NSLOT + 128, :], zer.t[:], deps=zer.RD(), store=True)
                        tok = None
                        for n in range(32):
                            prb = pr[n // 16]
                            c0 = (n % 16) * 32
                            tok = K.op("pe", lambda e: e.matmul(prb.t[:, c0:c0 + 32], lhsT=tri.t[:, :], rhs=Aall.t[:, n, :], start=True, stop=(n == 0)),
                                       (tri.RD() + Aall.RD() + ones.RD() + pr[0].WR() + pr[1].WR()) if n == 0 else [])
                            for n2 in range(n):
                                tok = K.op("pe", lambda e: e.matmul(prb.t[:, c0:c0 + 32], lhsT=ones.t[:, :], rhs=Aall.t[:, n2, :], start=False, stop=(n2 == n - 1)), [])
                            if n % 16 == 15:
                                prb.w(tok)
                        for hb_ in range(2):
                            prb = pr[hb_]
                            tl = slice(hb_ * 16, (hb_ + 1) * 16)
                            dv = K.op("dve", lambda e: e.tensor_tensor(out=rk.t[:, :], in0=prb.t[:, :], in1=eoffr.t[:, :], op=ALU.add), prb.RD() + eoffr.RD() + [dv])
                            dv = K.op("dve", lambda e: e.tensor_scalar(out=okm.t[:, :], in0=prb.t[:, :], scalar1=float(CAP) - 0.5, scalar2=None, op0=ALU.is_lt), [dv])
                            prb.r(dv)
                            dv = K.op("dve", lambda e: e.tensor_tensor(out=rk.t[:, :], in0=rk.t[:, :], in1=okm.t[:, :], op=ALU.mult), [dv])
                            for k_, Ak in enumerate((A1all, A2all)):
                                Av = Ak.t[:, tl, :].rearrange("p a b -> p (a b)")
                                dv = K.op("dve", lambda e: e.tensor_tensor(out=tm.t[:, :], in0=Av, in1=rk.t[:, :], op=ALU.mult), [dv] + Ak.RD())
                                dv = K.op("dve", lambda e: e.tensor_reduce(out=s4.t[:, 0, :], in_=tm.t[:, :].rearrange("p (a b) -> p a b", b=32), axis=AX.X, op=ALU.add), [dv])
                                dv = K.op("dve", lambda e: e.tensor_tensor(out=tm.t[:, :], in0=Av, in1=okm.t[:, :], op=ALU.mult), [dv])
                                dv = K.op("dve", lambda e: e.tensor_reduce(out=s4.t[:, 1, :], in_=tm.t[:, :].rearrange("p (a b) -> p a b", b=32), axis=AX.X, op=ALU.add), [dv])
                                dv = K.op("dve", lambda e: e.tensor_scalar(out=s4.t[:, 2, :], in0=s4.t[:, 1, :], scalar1=-float(NSLOT), scalar2=float(NSLOT),
                                                                           op0=ALU.mult, op1=ALU.add), [dv])
                                dv = K.op("dve", lambda e: e.tensor_tensor(out=s4.t[:, 3, :], in0=s4.t[:, 2, :], in1=s4.t[:, 0, :], op=ALU.add), [dv])
                                dv = K.op("dve", lambda e: e.tensor_copy(out=dsti.t[:, tl, k_], in_=s4.t[:, 3, :]), [dv])
                                dv = K.op("dve", lambda e: e.tensor_copy(out=recs.t[:, tl, k_, 0], in_=tokid.t[:, tl]), [dv] + tokid.RD())
                                dv = K.op("dve", lambda e: e.tensor_tensor(out=recs.t[:, tl, k_, 1], in0=wts.t[:, tl, k_], in1=s4.t[:, 1, :], op=ALU.mult),
                                          [dv] + wts.RD())
                        dsti.w(dv); recs.w(dv)
                        chs = K.chan()
                        K.wait("pool", [z1, z2] + dsti.RD())
                        stk = None
                        for n in range(32):
                            for k_ in range(2):
                                ins = nc.gpsimd.indirect_dma_start(out=rec_s[:, :], out_offset=bass.IndirectOffsetOnAxis(ap=dsti.t[:, n, k_:k_ + 1], axis=0),
                                                                   in_=recs.t[:, n, k_, :], in_offset=None)
                                chs.cnt += 16
                                ins.then_inc(chs.sem, 16)
                            if n % 8 == 7:
                                K.wait("pool", [(chs.sem, chs.cnt, chs.key)])
                        stk = (chs.sem, chs.cnt, chs.key)
                        K.stores[chs.key] = stk

                        with ExitStack() as ee:
                            wgb = [Buf(sb(ee, "wgb%d" % i, [128, 16, 512], BF16), K.chan()) for i in range(2)]
                            wub = [Buf(sb(ee, "wub%d" % i, [128, 16, 512], BF16), K.chan()) for i in range(2)]
                            wdb = [Buf(sb(ee, "wdb%d" % i, [128, 4, D], BF16), K.chan()) for i in range(2)]
                            recb = [Buf(sb(ee, "recb%d" % i, [128, 3, 2], F32), K.chan()) for i in range(2)]
                            reci = [Buf(sb(ee, "reci%d" % i, [128, 3], I32)) for i in range(2)]
                            xg = [Buf(sb(ee, "xg%d" % i, [128, D], BF16), K.chan()) for i in range(6)]
                            xgT = Buf(sb(ee, "xgT", [128, 16, CAP], BF16))
                            sg = [Buf(sb(ee, "sg%d" % i, [128, CAP], F32)) for i in range(2)]
                            aT = Buf(sb(ee, "aT", [128, 4, CAP], BF16))
                            yb = [Buf(sb(ee, "yb%d" % i, [128, D], F32), K.chan()) for i in range(2)]
                            pgu = [Buf(ps(ee, "pgu%d" % i, [128, 512], F32)) for i in range(4)]
                            ptx = Buf(ps(ee, "ptx", [128, 8, 128], BF16))
                            stD = dict(pgu=0, xg=0, yb=0, ev=0)

                            def load_expert(e_, slot):
                                K.barrier("sp"); K.barrier("pool")
                                b = wgb[slot]
                                b.w(K.dma(b.ch, "pool", b.t[:], w_gate[e_].rearrange("(c p) n -> p c n", p=128), deps=b.WR()))
                                b = wub[slot]
                                b.w(K.dma(b.ch, "pool", b.t[:], w_up[e_].rearrange("(c p) n -> p c n", p=128), deps=b.WR()))
                                b = wdb[slot]
                                b.w(K.dma(b.ch, "pool", b.t[:], w_down[e_].rearrange("(c p) n -> p c n", p=128), deps=b.WR()))
                                b = recb[slot]
                                b.w(K.dma(b.ch, "sp", b.t[:], rec_s[e_ * CAP:(e_ + 1) * CAP, :].rearrange("(s p) two -> p s two", p=128), deps=b.WR() + [stk]))

                            def gather_expert(e_):
                                slot = e_ % 2
                                rb_, ri_ = recb[slot], reci[slot]
                                dv = K.op("dve", lambda e: e.tensor_copy(out=ri_.t[:, :], in_=rb_.t[:, :, 0]), rb_.RD() + ri_.WR())
                                ri_.w(dv); rb_.r(dv)
                                for st_ in range(3):
                                    xgb = xg[slot * 3 + st_]
                                    K.wait("pool", xgb.WR() + ri_.RD())
                                    K.barrier("pool")
                                    ins = nc.gpsimd.indirect_dma_start(out=xgb.t[:, :], out_offset=None, in_=h2_s[:, :],
                                                                       in_offset=bass.IndirectOffsetOnAxis(ap=ri_.t[:, st_:st_ + 1], axis=0))
                                    xgb.ch.cnt += 16
                                    ins.then_inc(xgb.ch.sem, 16)
                                    gt = (xgb.ch.sem, xgb.ch.cnt, xgb.ch.key)
                                    xgb.w(gt)
                                    ri_.r(gt)

                            load_expert(0, 0)
                            gather_expert(0)
                            for e_ in range(NEXP):
                                slot = e_ % 2
                                if e_ + 1 < NEXP:
                                    load_expert(e_ + 1, (e_ + 1) % 2)
                                    gather_expert(e_ + 1)
                                rb_, ri_ = recb[slot], reci[slot]
                                for st_ in range(3):
                                    xgb = xg[slot * 3 + st_]
                                    for j2 in range(2):
                                        tk = None
                                        for c8 in range(8):
                                            c = 8 * j2 + c8
                                            tk = K.op("pe", lambda e: e.transpose(out=ptx.t[:, c8, :], in_=xgb.t[:, c * 128:(c + 1) * 128], identity=ident.t[:]),
                                                      xgb.RD() + (ptx.WR() if c8 == 0 else []))
                                        ptx.w(tk)
                                        dst = xgT.t[:, 8 * j2:8 * j2 + 8, st_ * 128:(st_ + 1) * 128]
                                        first = (st_ == 0 and j2 == 0)
                                        if j2 == 0:
                                            tk2 = K.op("act", lambda e: e.activation(out=dst, in_=ptx.t[:], func=AF.Copy), ptx.RD() + (xgT.WR() if first else []))
                                        else:
                                            tk2 = K.op("dve", lambda e: e.tensor_copy(out=dst, in_=ptx.t[:]), ptx.RD())
                                        ptx.r(tk2)
                                        xgT.w(tk2, first=first)
                                    xgb.r(tk)
                                wg_, wu_, wd__ = wgb[slot], wub[slot], wdb[slot]
                                for cb in range(4):
                                    pg_ = pgu[stD["pgu"] % 4]; stD["pgu"] += 1
                                    pu_ = pgu[stD["pgu"] % 4]; stD["pgu"] += 1
                                    tok = None
                                    for c in range(16):
                                        tok = K.op("pe", lambda e: e.matmul(pg_.t[:, 0:CAP], lhsT=wg_.t[:, c, cb * 128:(cb + 1) * 128], rhs=xgT.t[:, c, :],
                                                                            start=(c == 0), stop=(c == 15)), (wg_.RD() + xgT.RD() + pg_.WR()) if c == 0 else [])
                                    pg_.w(tok)
                                    for c in range(16):
                                        tok = K.op("pe", lambda e: e.matmul(pu_.t[:, 0:CAP], lhsT=wu_.t[:, c, cb * 128:(cb + 1) * 128], rhs=xgT.t[:, c, :],
                                                                            start=(c == 0), stop=(c == 15)), (wu_.RD() + pu_.WR()) if c == 0 else [])
                                    pu_.w(tok)
                                    sgb = sg[cb % 2]
                                    a1 = K.op("act", lambda e: e.activation(out=sgb.t[:, :], in_=pg_.t[:, 0:CAP], func=AF.Silu), pg_.RD() + sgb.WR())
                                    sgb.w(a1); pg_.r(a1)
                                    dv = K.op("dve", lambda e: e.tensor_tensor(out=aT.t[:, cb, :], in0=pu_.t[:, 0:CAP], in1=sgb.t[:, :], op=ALU.mult),
                                              pu_.RD() + sgb.RD() + (aT.WR() if cb == 0 else []))
                                    pu_.r(dv); sgb.r(dv)
                                    aT.w(dv, first=(cb == 0))
                                wg_.r(tok); wu_.r(tok); xgT.r(tok)
                                pend = []

                                def flush_evac(fence):
                                    nonlocal_ev = None
                                    while pend:
                                        pd_, ybb, cs, st_ = pend.pop(0)
                                        wsc = rb_.t[:, st_, 1:2]
                                        if True:
                                            ev_ = K.op("act", lambda e: e.activation(out=ybb.t[:, cs * 512:(cs + 1) * 512], in_=pd_.t[:, :], func=AF.Copy, scale=wsc),
                                                       pd_.RD() + rb_.RD() + [fence] + (ybb.WR() if cs == 0 else []))
                                        else:
                                            ev_ = K.op("dve", lambda e: e.tensor_scalar(out=ybb.t[:, cs * 512:(cs + 1) * 512], in0=pd_.t[:, :], scalar1=wsc, scalar2=None, op0=ALU.mult),
                                                       pd_.RD() + rb_.RD() + [fence] + (ybb.WR() if cs == 0 else []))
                                        stD["ev"] += 1
                                        pd_.r(ev_)
                                        ybb.w(ev_, first=(cs == 0))
                                        rb_.r(ev_)
                                        if cs == 3:
                                            row0 = e_ * CAP + st_ * 128
                                            stok = K.dma(ybb.ch, "sp", y_s[row0:row0 + 128, :], ybb.t[:], deps=ybb.RD(), store=True)
                                            ybb.r(stok)

                                for st_ in range(3):
                                    ybb = yb[stD["yb"] % 2]; stD["yb"] += 1
                                    for cs in range(4):
                                        pd_ = pgu[stD["pgu"] % 4]; stD["pgu"] += 1
                                        tok = None
                                        for ec_ in range(4):
                                            tok = K.op("pe", lambda e: e.matmul(pd_.t[:, :], lhsT=aT.t[:, ec_, st_ * 128:(st_ + 1) * 128], rhs=wd__.t[:, ec_, cs * 512:(cs + 1) * 512],
                                                                                start=(ec_ == 0), stop=(ec_ == 3)), (aT.RD() + wd__.RD() + pd_.WR()) if ec_ == 0 else [])
                                        pd_.w(tok)
                                        flush_evac(tok)
                                        pend.append((pd_, ybb, cs, st_))
                                fz = K.op("pe", lambda e: e.matmul(pr[0].t[:, 0:32], lhsT=tri.t[:, :], rhs=Aall.t[:, 0, :], start=True, stop=True), pr[0].WR())
                                pr[0].w(fz)
                                flush_evac(fz)
                                aT.r(tok); wd__.r(tok)
                            endE = [(K.sem[e], K.cnt[e], e) for e in ["pe", "act", "dve"]]
                            for e in ["pe", "act", "dve", "pool", "sp"]:
                                K.wait(e, endE)
                                K.barrier(e)

                        with ExitStack() as ef_:
                            xf = [Buf(sb(ef_, "xf%d" % i, [128, D], F32), K.chan()) for i in range(3)]
                            y1 = [Buf(sb(ef_, "y1%d" % i, [128, D], F32), K.chan()) for i in range(3)]
                            y2 = [Buf(sb(ef_, "y2%d" % i, [128, D], F32), K.chan()) for i in range(3)]
                            ob_ = [Buf(sb(ef_, "ofin%d" % i, [128, D], F32), K.chan()) for i in range(2)]
                            jk = Buf(sb(ef_, "jk", [128, D], BF16))
                            sf = Buf(sb(ef_, "sf", [128, 32], F32))
                            K.barrier("sp"); K.barrier("pool")

                            def cmb_load(n):
                                s_ = n % 3
                                rows = slice(n * 128, (n + 1) * 128)
                                xb, ya, yc = xf[s_], y1[s_], y2[s_]
                                xb.w(K.dma(xb.ch, "sp", xb.t[:], x1_s[rows, :], deps=xb.WR()))
                                for k_, yy in enumerate((ya, yc)):
                                    K.wait("pool", yy.WR())
                                    ins = nc.gpsimd.indirect_dma_start(out=yy.t[:, :], out_offset=None, in_=y_s[:, :],
                                                                       in_offset=bass.IndirectOffsetOnAxis(ap=dsti.t[:, n, k_:k_ + 1], axis=0))
                                    yy.ch.cnt += 16
                                    ins.then_inc(yy.ch.sem, 16)
                                    yy.w((yy.ch.sem, yy.ch.cnt, yy.ch.key))

                            def cmb_proc(n):
                                s_ = n % 3
                                rows = slice(n * 128, (n + 1) * 128)
                                xb, ya, yc, oo = xf[s_], y1[s_], y2[s_], ob_[n % 2]
                                p1 = K.op("pool", lambda e: e.tensor_tensor(out=ya.t[:], in0=ya.t[:], in1=yc.t[:], op=ALU.add), ya.RD() + yc.RD())
                                yc.r(p1)
                                dv = K.op("dve", lambda e: e.tensor_tensor(out=xb.t[:], in0=xb.t[:], in1=ya.t[:], op=ALU.add), [p1] + xb.RD())
                                ya.r(dv); ya.r(p1)
                                a1 = K.op("act", lambda e: e.activation(out=jk.t[:], in_=xb.t[:], func=AF.Square, accum_out=sf.t[:, n:n + 1]), [dv] + jk.WR())
                                a1 = K.op("act", lambda e: e.activation(out=sf.t[:, n:n + 1], in_=sf.t[:, n:n + 1], func=AF.Sqrt, scale=1.0 / D, bias=EPSC.t[:, 0:1]), [a1])
                                jk.w(a1)
                                dv = K.op("dve", lambda e: e.reciprocal(out=sf.t[:, n:n + 1], in_=sf.t[:, n:n + 1]), [a1])
                                dv = K.op("dve", lambda e: e.scalar_tensor_tensor(out=oo.t[:], in0=xb.t[:], scalar=sf.t[:, n:n + 1], in1=gfr.t[:], op0=ALU.mult, op1=ALU.mult),
                                          [dv] + oo.WR() + gfr.RD())
                                xb.r(dv); oo.w(dv)
                                stok = K.dma(oo.ch, "sp", out_d[rows, :], oo.t[:], deps=[dv], store=True)
                                oo.r(stok)

                            cmb_load(0)
                            cmb_load(1)
                            for n in range(32):
                                if n + 2 < 32:
                                    cmb_load(n + 2)
                                cmb_proc(n)

        K.barrier("sp")
    return nc


def make_tables():
    sm, sd = _slopes()
    sm = sm.astype(np.float32).astype(np.float64)
    sd = sd.astype(np.float32).astype(np.float64)
    j = np.arange(128)[:, None, None].astype(np.float64)
    t = {}
    t["ident"] = np.eye(128, dtype=np.float32).astype(ml_dtypes.bfloat16)
    t["identf"] = np.eye(128, dtype=np.float32)
    m = np.arange(64)[None, None, :]
    t["ab"] = (sm[None, :, None] * (j - 128.0 * m)).reshape(128, -1).astype(np.float32)
    u = np.arange(2)[None, None, :]
    t["ef"] = np.exp(-sm[None, :, None] * (u * 128.0 + j)).reshape(128, -1).astype(np.float32)
    qi = np.arange(256)[None, None, :]
    dl = qi - j
    t["bown"] = np.where(dl >= 0, -sm[None, :, None] * dl, NEG).reshape(128, -1).astype(np.float32)
    def cmult(dl):
        return ((dl >= 0) & (dl <= 128)).astype(np.float64) + ((dl >= 0) & (dl % 4 == 0) & (dl <= 512)) + \
               ((dl >= 0) & (dl % 16 == 0) & (dl <= 2048))
    jj = np.arange(128)[:, None]
    qq = np.arange(256)[None, :]
    bownd = np.empty((128, NH, 384), np.float32)
    for h in range(NH):
        for (c0, c1, dd, q0) in ((0, 256, 0, 0), (256, 384, -1, 128)):
            dl = (dd * 128 + qq[:, q0:q0 + (c1 - c0)] - jj).astype(np.int64)
            c = cmult(dl)
            with np.errstate(divide="ignore"):
                bownd[:, h, c0:c1] = np.where(c > 0, -sd[h] * dl + np.log(np.maximum(c, 1e-30)), NEG)
    t["bownd"] = bownd.reshape(128, -1)
    ddv = np.arange(17)[None, None, :]
    t["abd"] = (sd[None, :, None] * (j - 128.0 * ddv)).reshape(128, -1).astype(np.float32)
    t["efd"] = np.exp(-sd[None, :, None] * (u * 128.0 + j)).reshape(128, -1).astype(np.float32)
    def bf(x):
        return np.asarray(x, np.float32).astype(ml_dtypes.bfloat16)
    ctl = np.zeros((128, 3, 512), np.float32)
    for ci, pp in enumerate((0, 1, 7)):
        for kk in range(2):
            dd = 2 * pp + 2 - kk
            dl = (dd * 128 + qq - jj).astype(np.int64)
            ctl[:, ci, kk * 256:(kk + 1) * 256] = cmult(dl)
    t["ctl"] = bf(ctl.reshape(128, -1))
    lsd = np.zeros((128, 2, 128), np.float32)
    for s_ in range(16):
        lsd[s_, :, (np.arange(128) % 16) == s_] = 1.0
        lsd[16 + s_, :, (np.arange(128) % 16) == s_] = 1.0
    lsd[32, 1, :] = 1.0
    lsd[64, 1, :] = 1.0
    t["lsd"] = bf(lsd.reshape(128, -1))
    rg = np.zeros((NH, 128, 8, 2, 256), np.float32)
    for pp in range(8):
        if pp in (0, 1, 7):
            continue
        for kk in range(2):
            dd = 2 * pp + 2 - kk
            for s_ in range(16):
                dl = (dd * 128 + np.arange(256) - s_).astype(np.int64)
                c = cmult(dl)
                with np.errstate(divide="ignore"):
                    g = np.where(c > 0, np.log(np.maximum(c, 1e-30)), NEG).astype(np.float32)
                hi = bf(g).astype(np.float32)
                lo = bf(g - hi).astype(np.float32)
                rg[:, s_, pp, kk, :] = hi
                rg[:, 16 + s_, pp, kk, :] = lo
    for h in range(NH):
        v = np.float32(sd[h] * 128.0)
        hi = np.float32(bf(v)); lo = np.float32(bf(np.float32(v - hi)))
        rg[h, 32, :, :, :] = hi
        rg[h, 64, :, :, :] = lo
    t["rg"] = bf(rg.reshape(NH, 128, -1))
    es = np.zeros((128, 2, 32, 128), np.float32)
    for lb_ in range(32):
        es[lb_, :, lb_, :] = 1.0
    es[32, 1, :, :] = 1.0
    es[64, 1, :, :] = 1.0
    t["esel"] = es.reshape(128, -1).astype(ml_dtypes.bfloat16)
    sl = np.zeros((NH, 2, OWN), np.float32)
    for h in range(NH):
        v = np.float32(sm[h] * 128.0)
        hi = np.float32(v.astype(ml_dtypes.bfloat16))
        lo = np.float32(np.float32(v - hi).astype(ml_dtypes.bfloat16))
        sl[h, 0, :] = hi
        sl[h, 1, :] = lo
    t["slp"] = sl.astype(ml_dtypes.bfloat16)
    t["tri"] = np.triu(np.ones((128, 128), np.float32), 1).astype(ml_dtypes.bfloat16)
    t["eoff"] = np.tile((np.arange(32) * CAP).astype(np.float32)[None, :], (128, 1))
    t["eoffr"] = np.tile(t["eoff"], (1, 16))
    t["tokid"] = (np.arange(32)[None, :] * 128 + np.arange(128)[:, None]).astype(np.int32)
    return t


def make_core_inputs(inputs, tabs, core):
    b, r = core // 2, core % 2
    x = np.asarray(inputs["x"], np.float32)
    m = dict(tabs)
    if r == 0:
        xl = np.concatenate([np.zeros((256, D), np.float32), x[b, :S - 256]], axis=0)
    else:
        xl = x[b]
    m["xl"] = np.ascontiguousarray(xl)
    m["w_in"] = np.ascontiguousarray(inputs["w_in"][0], np.float32)
    m["g1rep"] = np.ascontiguousarray(np.tile(np.asarray(inputs["norm1_g"][0], np.float32)[None, :], (128, 1)))
    m["bg"] = np.ascontiguousarray(np.asarray(inputs["b_gates"][0], np.float32).reshape(32, 128).T)
    i = np.arange(16)[:, None]
    lb = np.arange(32)[None, :]
    ok = (lb <= 2 * i) & ((r == 1) | (lb >= 1))
    okq = np.repeat(ok, 2, axis=0)
    m["pbq"] = np.ascontiguousarray(np.tile(np.where(okq, 0.0, -1e30).astype(np.float32).reshape(1, -1), (128, 1)))
    m["pvq"] = np.ascontiguousarray(np.tile(okq.astype(np.float32).reshape(1, -1), (128, 1)))
    valid = np.ones((128, NT), np.float32)
    if r == 0:
        valid[:, 0:2] = 0.0
    m["valid"] = valid.astype(ml_dtypes.bfloat16)
    m["w_om"] = np.ascontiguousarray(inputs["w_out_moba"][0], np.float32)
    m["w_od"] = np.ascontiguousarray(inputs["w_out_dil"][0], np.float32)
    m["w_o"] = np.ascontiguousarray(inputs["w_o"][0], np.float32)
    m["g2rep"] = np.ascontiguousarray(np.tile(np.asarray(inputs["norm2_g"][0], np.float32)[None, :], (128, 1)))
    m["gfrep"] = np.ascontiguousarray(np.tile(np.asarray(inputs["norm_f_g"], np.float32)[None, :], (128, 1)))
    m["g2col"] = np.ascontiguousarray(np.asarray(inputs["norm2_g"][0], np.float32).reshape(16, 128).T)
    m["wr"] = np.ascontiguousarray(np.concatenate([inputs["w_group"][0], inputs["w_expert"][0]], axis=1), np.float32)
    m["rb"] = np.ascontiguousarray(np.tile(np.concatenate([inputs["b_group"][0], inputs["b_expert"][0]])[None, :], (128, 1)), np.float32)
    m["w_gate"] = np.ascontiguousarray(inputs["w_gate"][0], np.float32)
    m["w_up"] = np.ascontiguousarray(inputs["w_up"][0], np.float32)
    m["w_down"] = np.ascontiguousarray(inputs["w_down"][0], np.float32)
    return m


_NC_CACHE = {}


def kernel(**inputs):
    if "nc" not in _NC_CACHE:
        _NC_CACHE["nc"] = build_program()
    nc = _NC_CACHE["nc"]
    tabs = make_tables()
    in_maps = [make_core_inputs(inputs, tabs, c) for c in range(8)]
    res = run_bass_kernel_spmd(nc, in_maps, core_ids=list(range(8)))
    out = np.empty((4, S, D), np.float32)
    for c in range(8):
        b, r = c // 2, c % 2
        o = np.asarray(res.results[c]["out"]).reshape(16, 256, D)
        ov = out[b].reshape(32, 256, D)
        ov[r::2] = o
    return out
```
